# Optimizing a Trainium2 kernel written in Bass

```python
import jax
import jax.numpy as jnp
from jax import lax
import numpy as np

D_MODEL = 1024
BATCH = 32
SEQ = 2048
DEPTH = 2

MEM_LEN = 256
HEAD_DIM = 64
N_HEADS_A = 6
N_HEADS_B = 6
N_HEADS_M = 4
WIDTH_A = N_HEADS_A * HEAD_DIM
WIDTH_B = N_HEADS_B * HEAD_DIM
WIDTH_M = N_HEADS_M * HEAD_DIM
MIX_WIDTH = WIDTH_A + WIDTH_B + WIDTH_M
IN_WIDTH = 3 * WIDTH_A + 3 * WIDTH_B + WIDTH_M
DILATED_PATTERNS = ((128, 1), (512, 4), (2048, 16))
DIL_BLOCK = 64
GRID_W = 64
NA_ROWS = 8
NA_COLS = 16
N_GROUPS = 4
EXPERTS_PER_GROUP = 8
N_EXPERTS = N_GROUPS * EXPERTS_PER_GROUP
TOP_K_IN_GROUP = 2
D_EXPERT = 512
EPS = 1e-6
NEG_INF = -1e30
ATTN_SCALE = HEAD_DIM ** -0.5

kernel_name = "hybrid_dilated_neighbourhood_memory_hmoe_encoder"


def rms_norm(x, gain):
    xf = x.astype(jnp.float32)
    y = xf * lax.rsqrt(jnp.mean(jnp.square(xf), axis=-1, keepdims=True) + EPS)
    return (y * gain.astype(jnp.float32)).astype(x.dtype)


def alibi_slopes(n):
    return (2.0 ** (-8.0 * np.arange(1, n + 1) / n)).astype(np.float32)


def dilated_window_attention(q, k, v, slopes, window, dilation):
    b, s, h, dh = q.shape
    radius = window // (2 * dilation)
    length = s // dilation
    n_blk = -(-length // DIL_BLOCK)
    pad = n_blk * DIL_BLOCK - length

    def to_strided(t):
        t = t.reshape(b, length, dilation, h, dh).transpose(0, 2, 3, 1, 4)
        return jnp.pad(t, ((0, 0), (0, 0), (0, 0), (0, pad), (0, 0)))

    def band(t):
        t = jnp.pad(t, ((0, 0), (0, 0), (0, 0), (DIL_BLOCK, DIL_BLOCK), (0, 0)))
        t = t.reshape(b, dilation, h, n_blk + 2, DIL_BLOCK, dh)
        return jnp.concatenate([t[:, :, :, :-2], t[:, :, :, 1:-1], t[:, :, :, 2:]], axis=4)

    q_blk = to_strided(q).reshape(b, dilation, h, n_blk, DIL_BLOCK, dh)
    k_blk = band(to_strided(k))
    v_blk = band(to_strided(v))
    q_pos = jnp.arange(n_blk * DIL_BLOCK).reshape(n_blk, DIL_BLOCK)
    k_pos = (jnp.arange(n_blk)[:, None] - 1) * DIL_BLOCK + jnp.arange(3 * DIL_BLOCK)[None, :]
    rel = jnp.abs(k_pos[:, None, :] - q_pos[:, :, None])
    valid = (rel <= radius) & (k_pos[:, None, :] >= 0) & (k_pos[:, None, :] < length)
    alibi = -slopes[:, None, None, None] * (rel * dilation).astype(jnp.float32)[None]
    scores = jnp.einsum("bdhnqc,bdhnkc->bdhnqk", q_blk, k_blk).astype(jnp.float32) * ATTN_SCALE + alibi
    scores = jnp.where(valid, scores, NEG_INF)
    m = jnp.max(scores, axis=-1, keepdims=True)
    e = jnp.exp(scores - m)
    den = jnp.sum(e, axis=-1, keepdims=True)
    lse = (m + jnp.log(den))[..., 0]
    out = jnp.einsum("bdhnqk,bdhnkc->bdhnqc", e / den, v_blk.astype(jnp.float32))
    out = out.reshape(b, dilation, h, n_blk * DIL_BLOCK, dh)[:, :, :, :length]
    out = out.transpose(0, 3, 1, 2, 4).reshape(b, s, h, dh)
    lse = lse.reshape(b, dilation, h, n_blk * DIL_BLOCK)[..., :length]
    lse = lse.transpose(0, 3, 1, 2).reshape(b, s, h)
    return out, lse


def dilated_mixture_attention(q, k, v, slopes):
    outs, lses = [], []
    for window, dilation in DILATED_PATTERNS:
        o, l = dilated_window_attention(q, k, v, slopes, window, dilation)
        outs.append(o)
        lses.append(l)
    weights = jax.nn.softmax(jnp.stack(lses, axis=0), axis=0)
    return jnp.sum(weights[..., None] * jnp.stack(outs, axis=0), axis=0)


def neighbourhood_attention(q, k, v, rpb):
    b, s, h, dh = q.shape
    rows = s // GRID_W
    kh = min(NA_ROWS, rows)

    def to_grid(t):
        return t.reshape(b, rows, GRID_W, h, dh).transpose(0, 3, 1, 2, 4)

    qg, kg, vg = to_grid(q), to_grid(k), to_grid(v)
    r = jnp.arange(rows)
    r0 = jnp.clip(r - kh // 2, 0, rows - kh)
    row_idx = r0[:, None] + jnp.arange(kh)[None, :]
    k_rows = kg[:, :, row_idx]
    v_rows = vg[:, :, row_idx]
    c = jnp.arange(GRID_W)
    c0 = jnp.clip(c - NA_COLS // 2, 0, GRID_W - NA_COLS)
    col_ok = (c[None, :] >= c0[:, None]) & (c[None, :] < c0[:, None] + NA_COLS)
    dr = row_idx - r[:, None]
    dc = jnp.clip(c[None, :] - c[:, None], -(NA_COLS - 1), NA_COLS - 1)
    bias = rpb[:, dr[:, :, None, None] + NA_ROWS - 1, dc[None, None] + NA_COLS - 1]
    bias = bias.astype(jnp.float32).transpose(0, 1, 3, 2, 4)
    scores = jnp.einsum("bhrqc,bhrjkc->bhrqjk", qg, k_rows).astype(jnp.float32) * ATTN_SCALE + bias[None]
    scores = jnp.where(col_ok[None, None, None, :, None, :], scores, NEG_INF)
    probs = jax.nn.softmax(scores.reshape(b, h, rows, GRID_W, kh * GRID_W), axis=-1).reshape(scores.shape)
    out = jnp.einsum("bhrqjk,bhrjkc->bhrqc", probs, v_rows.astype(jnp.float32))
    return out.transpose(0, 2, 3, 1, 4).reshape(b, s, h, dh)


def memory_attention(q, k, v):
    scores = jnp.einsum("bshc,bmhc->bhsm", q, k).astype(jnp.float32) * ATTN_SCALE
    probs = jax.nn.softmax(scores, axis=-1)
    return jnp.einsum("bhsm,bmhc->bshc", probs, v.astype(jnp.float32))


def hierarchical_moe(h, w_group, b_group, w_router, b_router, w_gate, w_up, w_down):
    b, s, d = h.shape
    t = h.reshape(b * s, d)
    group_logits = jnp.einsum("td,dg->tg", t, w_group).astype(jnp.float32) + b_group.astype(jnp.float32)
    group_prob = jax.nn.softmax(group_logits, axis=-1)
    g_top, g_idx = lax.top_k(group_prob, 1)
    exp_logits = jnp.einsum("td,de->te", t, w_router).astype(jnp.float32) + b_router.astype(jnp.float32)
    exp_logits = exp_logits.reshape(-1, N_GROUPS, EXPERTS_PER_GROUP)
    in_group = jnp.take_along_axis(exp_logits, g_idx[:, :, None], axis=1)[:, 0]
    e_val, e_idx = lax.top_k(in_group, TOP_K_IN_GROUP)
    e_w = jax.nn.softmax(e_val, axis=-1) * g_top
    global_idx = g_idx * EXPERTS_PER_GROUP + e_idx
    gates = jnp.sum(jax.nn.one_hot(global_idx, N_EXPERTS, dtype=jnp.float32) * e_w[..., None], axis=1)
    gates = gates.astype(t.dtype)
    out = jnp.zeros_like(t)
    for e in range(N_EXPERTS):
        hid = jax.nn.silu(t @ w_gate[e]) * (t @ w_up[e])
        out = out + gates[:, e:e + 1] * (hid @ w_down[e])
    return out.reshape(b, s, d)


def setup_inputs(seed: int = 0) -> dict:
    key = jax.random.key(seed)
    ks = jax.random.split(key, 18)
    f32 = jnp.float32

    def nrm(k, shape, scale):
        return scale * jax.random.normal(k, shape, f32)

    return {
        "x": nrm(ks[0], (BATCH, SEQ, D_MODEL), 1.0),
        "mem": nrm(ks[1], (BATCH, MEM_LEN, D_MODEL), 1.0),
        "norm_mix": 1.0 + nrm(ks[2], (DEPTH, D_MODEL), 0.05),
        "w_in": nrm(ks[3], (DEPTH, D_MODEL, IN_WIDTH), D_MODEL ** -0.5),
        "qk_gain": 1.0 + nrm(ks[4], (DEPTH, 6, HEAD_DIM), 0.05),
        "rpb": nrm(ks[5], (DEPTH, N_HEADS_B, 2 * NA_ROWS - 1, 2 * NA_COLS - 1), 0.5),
        "norm_mem": 1.0 + nrm(ks[6], (DEPTH, D_MODEL), 0.05),
        "w_mem_kv": nrm(ks[7], (DEPTH, D_MODEL, 2 * WIDTH_M), D_MODEL ** -0.5),
        "out_gain": 1.0 + nrm(ks[8], (DEPTH, MIX_WIDTH), 0.05),
        "w_out": nrm(ks[9], (DEPTH, MIX_WIDTH, D_MODEL), MIX_WIDTH ** -0.5),
        "norm_ffn": 1.0 + nrm(ks[10], (DEPTH, D_MODEL), 0.05),
        "w_group": nrm(ks[11], (DEPTH, D_MODEL, N_GROUPS), D_MODEL ** -0.5),
        "b_group": nrm(ks[12], (DEPTH, N_GROUPS), 0.01),
        "w_router": nrm(ks[13], (DEPTH, D_MODEL, N_EXPERTS), D_MODEL ** -0.5),
        "b_router": nrm(ks[14], (DEPTH, N_EXPERTS), 0.01),
        "w_gate": nrm(ks[15], (DEPTH, N_EXPERTS, D_MODEL, D_EXPERT), D_MODEL ** -0.5),
        "w_up": nrm(ks[16], (DEPTH, N_EXPERTS, D_MODEL, D_EXPERT), D_MODEL ** -0.5),
        "w_down": nrm(ks[17], (DEPTH, N_EXPERTS, D_EXPERT, D_MODEL), D_EXPERT ** -0.5),
    }


def reference(x, mem, norm_mix, w_in, qk_gain, rpb, norm_mem, w_mem_kv, out_gain, w_out,
              norm_ffn, w_group, b_group, w_router, b_router, w_gate, w_up, w_down):
    bsz, seq, _ = x.shape
    mem_len = mem.shape[1]
    slopes = jnp.asarray(alibi_slopes(N_HEADS_A))
    split_at = [3 * WIDTH_A, 3 * WIDTH_A + 3 * WIDTH_B]
    for l in range(DEPTH):
        h = rms_norm(x, norm_mix[l])
        proj = jnp.einsum("bsd,de->bse", h, w_in[l])
        a_qkv, b_qkv, m_q = jnp.split(proj, split_at, axis=-1)
        qa, ka, va = (t.reshape(bsz, seq, N_HEADS_A, HEAD_DIM) for t in jnp.split(a_qkv, 3, axis=-1))
        qn, kn, vn = (t.reshape(bsz, seq, N_HEADS_B, HEAD_DIM) for t in jnp.split(b_qkv, 3, axis=-1))
        qm = m_q.reshape(bsz, seq, N_HEADS_M, HEAD_DIM)
        mem_kv = jnp.einsum("bmd,de->bme", rms_norm(mem, norm_mem[l]), w_mem_kv[l])
        km, vm = (t.reshape(bsz, mem_len, N_HEADS_M, HEAD_DIM) for t in jnp.split(mem_kv, 2, axis=-1))

        o_a = dilated_mixture_attention(rms_norm(qa, qk_gain[l, 0]), rms_norm(ka, qk_gain[l, 1]), va, slopes)
        o_b = neighbourhood_attention(rms_norm(qn, qk_gain[l, 2]), rms_norm(kn, qk_gain[l, 3]), vn, rpb[l])
        o_m = memory_attention(rms_norm(qm, qk_gain[l, 4]), rms_norm(km, qk_gain[l, 5]), vm)

        gain = out_gain[l]
        mixed = jnp.concatenate([
            rms_norm(o_a.reshape(bsz, seq, WIDTH_A), gain[:WIDTH_A]).astype(x.dtype),
            rms_norm(o_b.reshape(bsz, seq, WIDTH_B), gain[WIDTH_A:WIDTH_A + WIDTH_B]).astype(x.dtype),
            rms_norm(o_m.reshape(bsz, seq, WIDTH_M), gain[WIDTH_A + WIDTH_B:]).astype(x.dtype),
        ], axis=-1)
        x = x + jnp.einsum("bse,ed->bsd", mixed, w_out[l])

        h = rms_norm(x, norm_ffn[l])
        x = x + hierarchical_moe(h, w_group[l], b_group[l], w_router[l], b_router[l],
                                 w_gate[l], w_up[l], w_down[l])
    return x
```

```python
import contextlib
import numpy as np
import concourse.bass as bass
import concourse.mybir as mybir
from concourse.bass_utils import run_bass_kernel_spmd

F32 = mybir.dt.float32
BF16 = mybir.dt.bfloat16
AF = mybir.ActivationFunctionType
ALU = mybir.AluOpType
AX = mybir.AxisListType

D = 1024
S = 2048
NT = 16
MEM = 256
DEPTH = 2
NSEQ = 4
NE = 32
DE = 512
EPS = 1e-6
NEG = -30000.0
N_CORES = 8


class Sched:
    def __init__(self, nc, stack):
        self.nc = nc
        self.stack = stack
        self.eng = {}
        for name in ("pe", "act", "dve", "pool", "sp"):
            sem = stack.enter_context(nc.semaphore("s_" + name))
            self.eng[name] = dict(sem=sem, count=0, ops=[], seen={})
        self.res = {}
        self.dsem = {}
        self.guard = None
        self.gopened = set()

    def _need(self, eng, reads, writes):
        need = {}

        def add(ev, kind):
            sid, sem, val, src = ev
            if src == eng:
                if eng in ("pe", "sp"):
                    return
                if kind != "raw":
                    return
            if need.get(sid, (None, 0))[1] < val:
                need[sid] = (sem, val)

        for k in reads:
            r = self.res.get(k)
            if r and r["w"]:
                add(r["w"], "raw")
        for k in writes:
            r = self.res.get(k)
            if r:
                if r["w"]:
                    add(r["w"], "waw")
                for e in r["r"].values():
                    add(e, "war")
        return need

    def begin_guard(self, flag_ap, flag_key):
        self.guard = (flag_ap, flag_key)
        self.gopened = set()
        self.gsnap = {}

    def end_guard(self):
        for eng in self.gopened:
            self.eng[eng]["ops"].append(("gclose",))
            self.eng[eng]["seen"] = self.gsnap[eng]
        self.guard = None
        self.gopened = set()

    def op(self, eng, fn, reads=(), writes=(), dma_key=None, ndma=0):
        E = self.eng[eng]
        first_guarded = False
        if self.guard is not None and eng not in self.gopened:
            fneed = self._need(eng, [self.guard[1]], [])
            fw = []
            for sid, (sem, val) in fneed.items():
                if E["seen"].get(sid, 0) >= val:
                    continue
                E["seen"][sid] = val
                fw.append((sem, val))
            drain = [(E["sem"], E["count"])] if (E["count"] > 0 and eng != "sp") else []
            dtot = {k: (d[0], d[1]) for k, d in self.dsem.items()}
            E["ops"].append(("gopen", fw, self.guard[0], drain, dtot))
            self.gopened.add(eng)
            self.gsnap[eng] = dict(E["seen"])
            first_guarded = True
        need = self._need(eng, reads, writes)
        waits = []
        for sid, (sem, val) in need.items():
            if E["seen"].get(sid, 0) >= val:
                continue
            E["seen"][sid] = val
            waits.append((sem, val))
        if dma_key is None:
            E["count"] += 1
            ev = (eng, E["sem"], E["count"], eng)
            inc = E["sem"]
            rkey = eng
        else:
            d = self.dsem.get(dma_key)
            if d is None:
                sem = self.stack.enter_context(self.nc.semaphore("d_" + str(dma_key)))
                d = self.dsem[dma_key] = [sem, 0]
            d[1] += 16 * ndma
            ev = ("d_" + str(dma_key), d[0], d[1], "dma")
            inc = ("dma", d[0])
            rkey = "d_" + str(dma_key)
        E["ops"].append((waits, fn, inc, 16 * ndma))
        if first_guarded:
            self.res.setdefault(self.guard[1], dict(w=None, r={}))["r"]["g_" + eng] = ev
        for k in writes:
            self.res[k] = dict(w=ev, r={})
        for k in reads:
            self.res.setdefault(k, dict(w=None, r={}))["r"][rkey] = ev
        return ev

    def wait_all(self, eng, events):
        E = self.eng[eng]
        waits = []
        for sid, sem, val, src in events:
            if E["seen"].get(sid, 0) >= val:
                continue
            E["seen"][sid] = val
            waits.append((sem, val))
        E["ops"].append((waits, None, None, 0))

    def emit(self):
        nc = self.nc
        with nc.Block() as block:
            table = (("pe", block.tensor), ("act", block.scalar), ("dve", block.vector),
                     ("pool", block.gpsimd), ("sp", block.sync))
            for name, deco in table:
                ops = self.eng[name]["ops"]

                def body(e, ops=ops, name=name):
                    def real(rec):
                        waits, fn, inc, nd = rec
                        for sem, val in waits:
                            e.wait_ge(sem, val)
                        if fn is None:
                            return
                        if isinstance(inc, tuple):
                            fn(e, inc[1])
                        else:
                            fn(e).then_inc(inc, 1)

                    def ghost(rec):
                        waits, fn, inc, nd = rec
                        for sem, val in waits:
                            e.wait_ge(sem, val)
                        if fn is None:
                            return
                        if isinstance(inc, tuple):
                            e.sem_inc(inc[1], nd)
                        else:
                            e.sem_inc(inc, 1)

                    with e.register("gr_" + name) as greg:
                        i = 0
                        n = len(ops)
                        while i < n:
                            rec = ops[i]
                            if rec[0] == "gopen":
                                for sem, val in rec[1]:
                                    e.wait_ge(sem, val)
                                e.reg_load(greg, rec[2])
                                j = i + 1
                                blk = []
                                while ops[j][0] != "gclose":
                                    blk.append(ops[j])
                                    j += 1
                                with e.If_ne(greg, 0):
                                    for r in blk:
                                        real(r)
                                with e.Else():
                                    for sem, val in rec[3]:
                                        e.wait_ge(sem, val)
                                    nown = 0
                                    dadd = {}
                                    for r in blk:
                                        if r[1] is None:
                                            continue
                                        if isinstance(r[2], tuple):
                                            dadd[id(r[2][1])] = (r[2][1], dadd.get(id(r[2][1]), (None, 0))[1] + r[3])
                                        else:
                                            nown += 1
                                    for sem, add in dadd.values():
                                        for k, (dsem_h, tot) in rec[4].items():
                                            if dsem_h is sem and tot > 0:
                                                e.wait_ge(sem, tot)
                                        e.sem_inc(sem, add)
                                    if nown:
                                        e.sem_inc(self.eng[name]["sem"], nown)
                                i = j + 1
                                continue
                            real(rec)
                            i += 1

                deco(body)


def ssl(start, n, step):
    return slice(start, start + step * (n - 1) + 1, step)


def _alibi_slopes(n):
    return (2.0 ** (-8.0 * np.arange(1, n + 1) / n)).astype(np.float32)


def _bias_a():
    sl = _alibi_slopes(6)
    k = np.arange(128)[:, None]
    q = np.arange(128)[None, :]
    out = np.empty((6, 3, 128, 256), np.float32)
    for h in range(6):
        for p, dil in enumerate((1, 4, 16)):
            lo = np.where(k >= q, -sl[h] * dil * np.abs(k - q - 64).astype(np.float32), NEG)
            hi = np.where(k <= q, -sl[h] * dil * np.abs(k - q + 64).astype(np.float32), NEG)
            out[h, p, :, :128] = lo
            out[h, p, :, 128:] = hi
    return out


NA_VARIANTS = [(5, 5 + d) for d in (-2, -1, 0, 1, 2)] + \
              [(T, J) for T in (0, 1) for J in range(4)] + \
              [(T, J) for T in (14, 15) for J in range(12, 16)]


def na_variant(T, J):
    if 2 <= T <= 13:
        return J - T + 2
    if T < 2:
        return 5 + T * 4 + J
    return 13 + (T - 14) * 4 + (J - 12)


def na_keytiles(T):
    if T < 2:
        return list(range(4))
    if T > 13:
        return list(range(12, 16))
    return list(range(T - 2, T + 3))


def _bias_b(rpb):
    kl = np.arange(128)
    kr_off, kc = kl // 64, kl % 64
    qr_off, qc = kl // 64, kl % 64
    out = np.empty((DEPTH, 6, 128, len(NA_VARIANTS), 128), np.float32)
    for v, (T, J) in enumerate(NA_VARIANTS):
        r = (2 * T + qr_off)[None, :]
        keyrow = (2 * J + kr_off)[:, None]
        r0 = np.clip(r - 4, 0, 24)
        row_ok = (keyrow >= r0) & (keyrow < r0 + 8)
        c0 = np.clip(qc - 8, 0, 48)[None, :]
        col_ok = (kc[:, None] >= c0) & (kc[:, None] < c0 + 16)
        ok = row_ok & col_ok
        dr = np.clip(keyrow - r, -7, 7) + 7
        dc = np.clip(kc[:, None] - qc[None, :], -15, 15) + 15
        g = rpb[:, :, dr, dc]
        out[:, :, :, v, :] = np.where(ok[None, None], g, np.float32(NEG))
    return out


def _host_prep(inp):
    f = lambda a: np.ascontiguousarray(np.asarray(a, dtype=np.float32))
    w_in = f(inp["w_in"]).reshape(DEPTH, 8, 128, 20, 128).transpose(0, 3, 2, 1, 4)
    w_mem = f(inp["w_mem_kv"]).reshape(DEPTH, 8, 128, 4, 128).transpose(0, 3, 2, 1, 4)
    w_out = f(inp["w_out"]).reshape(DEPTH, 8, 128, 1024).transpose(0, 2, 1, 3)
    w_gate = f(inp["w_gate"]).reshape(DEPTH, NE, 8, 128, DE).transpose(0, 1, 3, 2, 4)
    w_up = f(inp["w_up"]).reshape(DEPTH, NE, 8, 128, DE).transpose(0, 1, 3, 2, 4)
    w_down = f(inp["w_down"]).reshape(DEPTH, NE, 4, 128, D).transpose(0, 1, 3, 2, 4)
    w_rt = np.concatenate([f(inp["w_group"]), f(inp["w_router"])], axis=2)
    w_rt = w_rt.reshape(DEPTH, 8, 128, 36).transpose(0, 2, 1, 3)
    b_rt = np.concatenate([f(inp["b_group"]), f(inp["b_router"])], axis=1)
    qkg = np.tile(f(inp["qk_gain"]), (1, 1, 2)).transpose(2, 0, 1).reshape(128, DEPTH * 6)
    og = f(inp["out_gain"]).reshape(DEPTH, 8, 128).transpose(2, 0, 1).reshape(128, DEPTH * 8)
    shared = dict(
        w_in=np.ascontiguousarray(w_in), w_mem=np.ascontiguousarray(w_mem),
        w_out=np.ascontiguousarray(w_out), w_gate=np.ascontiguousarray(w_gate),
        w_up=np.ascontiguousarray(w_up), w_down=np.ascontiguousarray(w_down),
        w_rt=np.ascontiguousarray(w_rt), b_rt=np.ascontiguousarray(b_rt),
        g_mix=f(inp["norm_mix"]), g_mem=f(inp["norm_mem"]), g_ffn=f(inp["norm_ffn"]),
        qkg=np.ascontiguousarray(qkg), og=np.ascontiguousarray(og),
        bias_a=_bias_a(), bias_b=_bias_b(f(inp["rpb"])),
        ident=np.eye(128, dtype=np.float32),
        triu=np.triu(np.ones((128, 128), np.float32), 1),
        eoff=np.tile((np.arange(NE, dtype=np.float32) * S)[None, :], (128, 1)),
        bones=np.kron(np.eye(2, dtype=np.float32), np.ones((64, 64), np.float32)),
    )
    return shared


def build_nc(nseq=NSEQ, depth=DEPTH, do_attn=True, do_moe=True, n_experts=NE, stop=None, sparse=True, guard=True):
    nc = bass.Bass("TRN2", target_bir_lowering=False)
    dr = {}

    def din(name, shape):
        dr[name] = nc.dram_tensor(name, list(shape), F32, kind="ExternalInput").ap()
        return dr[name]

    x_d = din("x", (nseq, S, D))
    mem_d = din("mem", (nseq, MEM, D))
    w_in_d = din("w_in", (DEPTH, 20, 128, 8, 128))
    w_mem_d = din("w_mem", (DEPTH, 4, 128, 8, 128))
    w_out_d = din("w_out", (DEPTH, 128, 8, 1024))
    if do_moe:
        w_gate_d = din("w_gate", (DEPTH, NE, 128, 8, DE))
        w_up_d = din("w_up", (DEPTH, NE, 128, 8, DE))
        w_down_d = din("w_down", (DEPTH, NE, 128, 4, D))
    w_rt_d = din("w_rt", (DEPTH, 128, 8, 36))
    b_rt_d = din("b_rt", (DEPTH, 36))
    g_mix_d = din("g_mix", (DEPTH, D))
    g_mem_d = din("g_mem", (DEPTH, D))
    g_ffn_d = din("g_ffn", (DEPTH, D))
    qkg_d = din("qkg", (128, DEPTH * 6))
    og_d = din("og", (128, DEPTH * 8))
    bias_a_d = din("bias_a", (6, 3, 128, 256))
    bias_b_d = din("bias_b", (DEPTH, 6, 128, 21, 128))
    ident_d = din("ident", (128, 128))
    triu_d = din("triu", (128, 128))
    eoff_d = din("eoff", (128, NE))
    I32 = mybir.dt.int32
    Hs_d = nc.dram_tensor("Hs_scr", [NE * S, D], BF16).ap()
    Ys_d = nc.dram_tensor("Ys_scr", [NE * S, D], F32).ap()
    bones_d = din("bones", (128, 128))
    y_d = nc.dram_tensor("y", [nseq, S, D], F32, kind="ExternalOutput").ap()

    stack = contextlib.ExitStack()
    with stack:
        def sb(name, shape, dt=F32):
            return stack.enter_context(nc.sbuf_tensor(name, list(shape), dt))

        UN = 43 * 1024
        U = sb("U", (128, UN), BF16)

        class Bump:
            def __init__(self):
                self.off = 0

            def alloc(self, shape, dt=F32):
                n = int(np.prod(shape[1:]))
                ne = n * 2 if dt == F32 else n
                ne = (ne + 1) // 2 * 2
                assert self.off + ne <= UN, ("union overflow", self.off, ne)
                v = U[:, self.off:self.off + ne]
                self.off += ne
                if dt == F32:
                    v = v.bitcast(F32)
                if len(shape) == 3:
                    v = v.rearrange("p (a b) -> p a b", a=shape[1])
                elif len(shape) == 4:
                    v = v.rearrange("p (a b c) -> p a b c", a=shape[1], b=shape[2])
                elif len(shape) == 5:
                    v = v.rearrange("p (a b c d) -> p a b c d", a=shape[1], b=shape[2], c=shape[3])
                return v

        x_sb = sb("x_sb", (128, NT, D))
        hT = sb("hT", (128, 8, S), BF16)
        sq = [sb("sq%d" % i, (128, 512), BF16) for i in range(2)]
        rs = [sb("rs%d" % i, (128, 512)) for i in range(2)]
        htok = [sb("htok%d" % i, (128, D), BF16) for i in range(2)]
        junk = sb("junk", (128, D), BF16)
        gbc = sb("gbc", (128, D))
        ssum = sb("ssum", (128, 32))
        rstd = sb("rstd", (128, 32))
        ident = sb("ident_sb", (128, 128), BF16)
        bones = sb("bones_sb", (128, 128), BF16)
        ones = sb("ones_sb", (128, 128), BF16)
        qkg = sb("qkg_sb", (128, DEPTH * 6))
        qkg8 = sb("qkg8_sb", (128, DEPTH * 6))
        og = sb("og_sb", (128, DEPTH * 8))
        Wr = sb("Wr", (128, 8, 36), BF16)
        brt = sb("brt", (128, 36))
        gates = sb("gates", (128, NT, NE))
        lg = sb("lg", (128, 36))
        rt_s = sb("rt_s", (128, 16))
        mx8 = sb("mx8", (128, 8))
        lmask = sb("lmask", (128, NE))
        eq1 = sb("eq1", (128, NE))
        eq2 = sb("eq2", (128, NE))
        triu = sb("triu_sb", (128, 128), BF16)
        eoff = sb("eoff_sb", (128, NE))
        Mall = sb("Mall", (128, NT, NE), BF16)
        slotf = sb("slotf", (128, NE))
        slot01f = sb("slot01f", (128, 2))
        slot01 = sb("slot01", (128, NT, 2), I32)
        g01 = sb("g01", (128, NT, 2))
        cntf = sb("cntf", (128, NE + 1))
        flag_i = sb("flag_i", (128, NE + 1), I32)
        ba = Bump()
        oT = ba.alloc((128, 3, S), BF16)
        acc = ba.alloc((128, S))
        qT = ba.alloc((128, S), BF16)
        kT = ba.alloc((128, S), BF16)
        vT = ba.alloc((128, S), BF16)
        VA = [ba.alloc((128, NT, 2, 128), BF16) for i in range(2)]
        Wt = [ba.alloc((128, 3, 8, 128), BF16) for i in range(1)]
        Wo = ba.alloc((128, 3, D), BF16)
        biasA = ba.alloc((128, 2, 3, 256), BF16)
        biasB = ba.alloc((128, 21, 128), BF16)
        PT = [ba.alloc((128, 512), BF16) for i in range(2)]
        mem_sb = acc.rearrange("p (a b) -> p a b", a=2)
        kmT = ba.alloc((128, 2, MEM), BF16)
        vmT = ba.alloc((128, MEM), BF16)
        VAM = ba.alloc((128, 2, 2, 2, 128), BF16)
        bm = Bump()
        Wg = [bm.alloc((128, 8, DE), BF16) for i in range(2)]
        Wu = [bm.alloc((128, 8, DE), BF16) for i in range(2)]
        Wd = [bm.alloc((128, 4, D), BF16) for i in range(2)]
        sg = [bm.alloc((128, 256 if sparse else 512), BF16) for i in range(2)]
        hid = [bm.alloc((128, 4, 256 if sparse else 512), BF16) for i in range(2)]
        if sparse:
            hTe = [bm.alloc((128, 8, 256), BF16) for i in range(2)]
            hs_tok = [bm.alloc((128, 2, D), BF16) for i in range(2)]
            yout = [bm.alloc((128, D)) for i in range(2)]
            Yg = [bm.alloc((128, D)) for i in range(2)]

        def ps(name, shape, dt=F32):
            return stack.enter_context(nc.psum_tensor(name, list(shape), dt))
        pA = [ps("pA%d" % i, (128, 512)) for i in range(3)]
        pTr = ps("pTr", (128, 8, 128), BF16)
        pY = [ps("pY%d" % i, (128, 1024)) for i in range(2)]
        B = [(pA[0], ("ps", 0)), (pA[1], ("ps", 1)), (pA[2], ("ps", 2))]
        BK_TR = ("ps", 3)
        Yk = [(("ps", 4), ("ps", 5)), (("ps", 6), ("ps", 7))]

        sc = Sched(nc, stack)
        op = sc.op

        def dma(queue, key, pairs, reads=(), writes=()):
            def fn(e, sem, pairs=pairs):
                for o, i in pairs:
                    e.dma_start(out=o, in_=i).then_inc(sem, 16)
            return op(queue, fn, reads=reads, writes=writes, dma_key=key, ndma=len(pairs))

        dma("pool", "cst", [(ident[:], ident_d), (bones[:], bones_d)], writes=["ident", "bones"])
        dma("sp", "cst2", [(qkg[:], qkg_d), (og[:], og_d), (eoff[:], eoff_d)], writes=["qkg", "og", "eoff"])
        dma("pool", "cst3", [(triu[:], triu_d)], writes=["triu"])
        op("pool", lambda e: e.memset(ones[:], 1.0), writes=["ones"])
        op("dve", lambda e: e.tensor_scalar_mul(qkg8[:], qkg[:], 0.125), reads=["qkg"], writes=["qkg8"])

        rr = dict(hte=0, hst=0, yo=0, yg=0, proj=0, st=0, pv=0, tr=0, y=0, pt=0, sq=0, rs=0, ht=0, wt=0, va=0, sg=0, hid=0, wm=0)

        def nxt(name, n=2):
            v = rr[name]
            rr[name] = (v + 1) % n
            return v

        def norm_to_hT(src, src_key, ntile, gain_d_row, dstT, dst_key):
            dma("sp", "gbc", [(gbc[:], gain_d_row.partition_broadcast(128))], writes=["gbc"])
            if stop == "n1":
                return
            for t in range(ntile):
                op("act", lambda e, t=t: e.activation(junk[:], src[:, t, :], AF.Square,
                                                      accum_out=ssum[:, t:t + 1]),
                   reads=[(src_key, t)], writes=["junk", ("ssum", t)])
                if stop == "n2":
                    continue
                op("act", lambda e, t=t: e.activation(rstd[:, t:t + 1], ssum[:, t:t + 1], AF.Ln,
                                                      bias=float(EPS), scale=1.0 / D),
                   reads=[("ssum", t)], writes=[("rstdq", t), ("rstd", t)])
                op("act", lambda e, t=t: e.activation(rstd[:, t:t + 1], rstd[:, t:t + 1], AF.Exp, scale=-0.5),
                   reads=[("rstdq", t)], writes=[("rstd", t)])
                if stop == "n3":
                    continue
                hb = nxt("ht")
                op("dve", lambda e, t=t, hb=hb: e.scalar_tensor_tensor(
                    htok[hb][:], src[:, t, :], rstd[:, t:t + 1], gbc[:], ALU.mult, ALU.mult),
                   reads=[(src_key, t), ("rstd", t), "gbc"], writes=[("htok", hb)])
                if stop == "n4":
                    continue

                def tr(e, hb=hb):
                    ins = None
                    for c in range(8):
                        ins = e.transpose(pTr[:, c, :], htok[hb][:, c * 128:(c + 1) * 128], ident[:])
                    return ins
                op("pe", tr, reads=[("htok", hb), "ident"], writes=[BK_TR])
                if stop == "n5":
                    continue
                op("act", lambda e, t=t: e.copy(dstT[:, :, t * 128:(t + 1) * 128], pTr[:]),
                   reads=[BK_TR], writes=[(dst_key, t)])

        def proj_chunk(W_ap, w_key, srcT, src_key, ntok, consume):
            nblk = (ntok + 511) // 512
            pend = None
            for b in range(nblk):
                n = min(512, ntok - b * 512)
                bi = nxt("proj")
                bank, bkey = B[bi]

                def mm(e, b=b, n=n, bank=bank):
                    ins = None
                    for kc in range(8):
                        ins = e.matmul(bank[:, 0:n], W_ap(kc), srcT[:, kc, b * 512:b * 512 + n],
                                       start=(kc == 0), stop=(kc == 7))
                    return ins
                tiles = [(src_key, t) for t in range(b * 4, b * 4 + (n + 127) // 128)]
                op("pe", mm, reads=[w_key] + tiles, writes=[bkey])
                if pend is not None:
                    consume(*pend)
                pend = (bank, bkey, b, n)
            if pend is not None:
                consume(*pend)

        def qk_consume(dst, dst_key, gcol_ap):
            def consume(bank, bkey, b, n):
                si = nxt("sq")
                op("act", lambda e: e.activation(sq[si][:, 0:n], bank[:, 0:n], AF.Square),
                   reads=[bkey], writes=[("sq", si)])
                sbank, skey = B[2]
                op("pe", lambda e: e.matmul(sbank[:, 0:n], bones[:], sq[si][:, 0:n], start=True, stop=True),
                   reads=[("sq", si), "bones"], writes=[skey])
                ri = nxt("rs")
                op("act", lambda e: e.activation(rs[ri][:, 0:n], sbank[:, 0:n], AF.Ln, bias=float(EPS), scale=1.0 / 64),
                   reads=[skey], writes=[("rsl", ri), ("rs", ri)])
                op("act", lambda e: e.activation(rs[ri][:, 0:n], rs[ri][:, 0:n], AF.Exp, scale=-0.5),
                   reads=[("rsl", ri)], writes=[("rs", ri)])
                op("dve", lambda e: e.scalar_tensor_tensor(dst[:, b * 512:b * 512 + n], bank[:, 0:n], gcol_ap,
                                                           rs[ri][:, 0:n], ALU.mult, ALU.mult),
                   reads=[bkey, ("rs", ri), "qkg", "qkg8"], writes=[(dst_key, b)])
            return consume

        def copy_consume(dst, dst_key):
            def consume(bank, bkey, b, n):
                op("act", lambda e: e.copy(dst[:, b * 512:b * 512 + n], bank[:, 0:n]),
                   reads=[bkey], writes=[(dst_key, b)])
            return consume

        def build_va(va, va_key, tile_tokens, src, src_keys):
            nt = len(tile_tokens)
            for g0 in range(0, nt, 8):
                g1 = min(nt, g0 + 8)

                def tr(e, g0=g0, g1=g1):
                    ins = None
                    for t in range(g0, g1):
                        ins = e.transpose(pTr[:, t - g0, :], src[:, tile_tokens[t]], ident[:])
                    return ins
                op("pe", tr, reads=list(src_keys) + ["ident"], writes=[BK_TR])
                op("dve", lambda e, g0=g0, g1=g1: e.tensor_copy(va[:, g0:g1, 0, 0:64], pTr[:, 0:g1 - g0, 0:64]),
                   reads=[BK_TR], writes=[va_key])
                op("dve", lambda e, g0=g0, g1=g1: e.tensor_copy(va[:, g0:g1, 1, 64:128], pTr[:, 0:g1 - g0, 64:128]),
                   reads=[BK_TR], writes=[va_key + ("b",)])

        def attention(qblocks, q_keys, k_keys, va_keys, bias_keys, acc_key):
            i = 0
            nq = len(qblocks)
            prev_stage2 = None
            while i < nq:
                cols = 0
                grp = []
                while i < nq and cols + qblocks[i]["n"] * len(qblocks[i]["items"]) <= 512:
                    grp.append(qblocks[i])
                    cols += qblocks[i]["n"] * len(qblocks[i]["items"])
                    i += 1
                    if len(grp) > 0 and i < nq and qblocks[i].get("flush_before"):
                        break
                assert grp, "qblock too large"
                sti = 1 - nxt("st")
                sbank, skey = (pY[0][:, sti * 512:(sti + 1) * 512], Yk[0][sti])
                pti = nxt("pt")

                def st_mm(e, grp=grp, sbank=sbank):
                    ins = None
                    c = 0
                    first = True
                    for qb in grp:
                        for (k_ap, v_ap, b_ap) in qb["items"]:
                            if b_ap is not None:
                                e.matmul(sbank[:, c:c + qb["n"]], ident[:], b_ap, start=first, stop=False,
                                         skip_group_check=True)
                                first = False
                            c += qb["n"]
                    c = 0
                    for qb in grp:
                        for (k_ap, v_ap, b_ap) in qb["items"]:
                            ins = e.matmul(sbank[:, c:c + qb["n"]], k_ap, qb["q"],
                                           start=(first and b_ap is None), stop=True, skip_group_check=True)
                            if b_ap is None:
                                first = False
                            c += qb["n"]
                    return ins
                op("pe", st_mm, reads=list(q_keys) + list(k_keys) + list(bias_keys) + ["ident"], writes=[skey])
                op("act", lambda e, cols=cols, sbank=sbank, pti=pti: e.activation(PT[pti][:, 0:cols], sbank[:, 0:cols], AF.Exp),
                   reads=[skey], writes=[("PT", pti)])
                def stage2(grp=grp, pti=pti):
                    pv_pending = []
                    c = 0
                    for qb in grp:
                        offs = []
                        for _ in qb["items"]:
                            offs.append(c)
                            c += qb["n"]
                        pv_pending.append((qb, pti, offs))
                    j = 0
                    while j < len(pv_pending):
                        tot = 0
                        piece = []
                        while j < len(pv_pending) and tot + pv_pending[j][0]["n"] <= 512:
                            piece.append(pv_pending[j])
                            tot += pv_pending[j][0]["n"]
                            j += 1
                        pvi = nxt("pv")
                        pbank, pkey = (pY[1][:, pvi * 512:(pvi + 1) * 512], Yk[1][pvi])

                        def pv_mm(e, piece=piece, pbank=pbank):
                            ins = None
                            c2 = 0
                            for (qb, pti2, offs) in piece:
                                for ii, (k_ap, v_ap, b_ap) in enumerate(qb["items"]):
                                    ins = e.matmul(pbank[:, c2:c2 + qb["n"]], v_ap,
                                                   PT[pti2][:, offs[ii]:offs[ii] + qb["n"]],
                                                   start=(ii == 0), stop=(ii == len(qb["items"]) - 1),
                                                   skip_group_check=True)
                                c2 += qb["n"]
                            return ins
                        op("pe", pv_mm, reads=[("PT", piece[0][1])] + list(va_keys), writes=[pkey])
                        c2 = 0
                        for (qb, pti2, offs) in piece:
                            n = qb["n"]
                            if qb["add"]:
                                op("dve", lambda e, qb=qb, c2=c2, n=n, pbank=pbank: e.tensor_tensor(
                                    qb["dst"], pbank[:, c2:c2 + n], qb["dst"], ALU.add),
                                   reads=[pkey, acc_key], writes=[acc_key])
                            else:
                                op("dve", lambda e, qb=qb, c2=c2, n=n, pbank=pbank: e.tensor_copy(
                                    qb["dst"], pbank[:, c2:c2 + n]),
                                   reads=[pkey], writes=[acc_key])
                            c2 += n
                    pv_pending = []

                if prev_stage2 is not None:
                    prev_stage2()
                prev_stage2 = stage2

            if prev_stage2 is not None:
                prev_stage2()

        def head_epilogue(hh, chunk_slot, acc_key, ntok=S):
            u = slice(0, 64) if hh == 0 else slice(64, 128)
            dn = slice(64, 128) if hh == 0 else slice(0, 64)
            for b in range(ntok // 512):
                cs = slice(b * 512, (b + 1) * 512)
                ri = nxt("rs")
                op("pool", lambda e, ri=ri, cs=cs: e.tensor_copy(rs[ri][u, :], acc[dn, cs]),
                   reads=[acc_key], writes=[("rsl", ri), ("rs", ri)])
                op("dve", lambda e, ri=ri: e.reciprocal(rs[ri][u, :], rs[ri][u, :]),
                   reads=[("rsl", ri)], writes=[("rs", ri)])
                op("dve", lambda e, ri=ri, cs=cs: e.tensor_tensor(oT[u, chunk_slot, cs], acc[u, cs], rs[ri][u, :], ALU.mult),
                   reads=[acc_key, ("rs", ri)], writes=[("oT", chunk_slot, hh)])

        def group_finalize(l, chunks, width):
            ng = len(chunks)
            dma("pool", "Wo", [(Wo[:, 0:ng, :], w_out_d[l, :, chunks[0]:chunks[0] + ng, :])], writes=["Wo"])
            for b in range(4):
                sbank, skey = B[2]
                for ci in range(ng):
                    si = nxt("sq")
                    op("act", lambda e, ci=ci, si=si, b=b: e.activation(sq[si][:], oT[:, ci, b * 512:(b + 1) * 512], AF.Square),
                       reads=[("oT", ci, 0), ("oT", ci, 1)], writes=[("sq", si)])
                    op("pe", lambda e, ci=ci, si=si: e.matmul(sbank[:], ones[:], sq[si][:], start=(ci == 0), stop=(ci == ng - 1)),
                       reads=[("sq", si), "ones"], writes=[skey])
                ri = nxt("rs")
                op("act", lambda e, ri=ri: e.activation(rs[ri][:], sbank[:], AF.Ln, bias=float(EPS), scale=1.0 / width),
                   reads=[skey], writes=[("rsl", ri), ("rs", ri)])
                op("act", lambda e, ri=ri: e.activation(rs[ri][:], rs[ri][:], AF.Exp, scale=-0.5),
                   reads=[("rsl", ri)], writes=[("rs", ri)])
                for ci in range(ng):
                    gcol = og[:, l * 8 + chunks[ci]:l * 8 + chunks[ci] + 1]
                    op("dve", lambda e, ci=ci, gcol=gcol, ri=ri, b=b: e.scalar_tensor_tensor(
                        oT[:, ci, b * 512:(b + 1) * 512], oT[:, ci, b * 512:(b + 1) * 512], gcol, rs[ri][:],
                        ALU.mult, ALU.mult),
                       reads=[("oT", ci, 0), ("oT", ci, 1), ("rs", ri), "og"], writes=[("mix", ci, b)])
            for t in range(NT):
                yi = nxt("y")

                def mm(e, t=t, yi=yi):
                    ins = None
                    for half in range(2):
                        for ci in range(ng):
                            ins = e.matmul(pY[yi][:, half * 512:(half + 1) * 512], oT[:, ci, t * 128:(t + 1) * 128],
                                           Wo[:, ci, half * 512:(half + 1) * 512], start=(ci == 0), stop=(ci == ng - 1))
                    return ins
                op("pe", mm, reads=[("mix", ci, t // 4) for ci in range(ng)] + ["Wo"], writes=list(Yk[yi]))
                op("dve", lambda e, t=t, yi=yi: e.tensor_tensor(x_sb[:, t, :], pY[yi][:], x_sb[:, t, :], ALU.add),
                   reads=list(Yk[yi]) + [("x", t)], writes=[("x", t)])
            for ci in range(ng):
                for hh in range(2):
                    sc.res[("oT", ci, hh)] = sc.res[("mix", ci, 3)]

        def load_pair_w(l, chunk_ids):
            wi = nxt("wt", len(Wt))
            dma("pool", "Wt%d" % wi, [(Wt[wi][:, j, :, :], w_in_d[l, cid]) for j, cid in enumerate(chunk_ids)],
                writes=[("Wt", wi)])
            return wi

        def attn_phase(s, l):
            for i in range(2):
                op("pool", lambda e, i=i: e.memset(VA[i], 1.0), writes=[("VA", i), ("VA", i, "b")])
            op("pool", lambda e: e.memset(VAM, 1.0), writes=["VAM", ("VAM", "b")])
            if stop == "memset":
                return
            mem_path(s, l)
            if stop == "mem" or (stop and stop[0] in "nm" and stop != "norm"):
                return
            norm_to_hT(x_sb, "x", NT, g_mix_d[l:l + 1, :], hT, "hT")
            if stop == "norm":
                return
            hkeys = [("hT", t) for t in range(NT)]
            for grp_name in ("A", "B", "M"):
                if grp_name == "M":
                    npair = 2
                else:
                    npair = 3
                for pr in range(npair):
                    if grp_name == "A":
                        cids = [pr, 3 + pr, 6 + pr]
                        gi = (0, 1)
                    elif grp_name == "B":
                        cids = [9 + pr, 12 + pr, 15 + pr]
                        gi = (2, 3)
                    else:
                        cids = [18 + pr]
                        gi = (4, 5)
                    wi = load_pair_w(l, cids)
                    proj_chunk(lambda kc, wi=wi: Wt[wi][:, 0, kc, :], ("Wt", wi), hT, "hT", S,
                               qk_consume(qT, "qT", qkg8[:, l * 6 + gi[0]:l * 6 + gi[0] + 1]))
                    qkeys = [("qT", b) for b in range(4)]
                    if grp_name != "M":
                        proj_chunk(lambda kc, wi=wi: Wt[wi][:, 1, kc, :], ("Wt", wi), hT, "hT", S,
                                   qk_consume(kT, "kT", qkg[:, l * 6 + gi[1]:l * 6 + gi[1] + 1]))
                        proj_chunk(lambda kc, wi=wi: Wt[wi][:, 2, kc, :], ("Wt", wi), hT, "hT", S,
                                   copy_consume(vT, "vT"))
                        kkeys = [("kT", b) for b in range(4)]
                        vkeys = [("vT", b) for b in range(4)]
                        hb = 0 if grp_name == "A" else 1
                        if grp_name == "A":
                            dma("pool", "biasA", [(biasA[:, hh, :, :], bias_a_d[2 * pr + hh].rearrange("p k q -> k p q"))
                                                  for hh in range(2)], writes=["biasA"])
                    if grp_name == "A":
                        for hh in range(2):
                            pass
                        accs = {}
                        for hh in range(2):
                            hs = slice(64 * hh, 64 * hh + 64)
                            for p, dil in enumerate((1, 4, 16)):
                                vi = p % 2 if hh == 0 else None
                        for hh in range(2):
                            hs = slice(64 * hh, 64 * hh + 64)
                            for p, dil in enumerate((1, 4, 16)):
                                L = S // dil
                                nkt = L // 128
                                vi = nxt("va")
                                va = VA[vi]
                                toks = [ssl(dil * 128 * j + r, 128, dil)
                                        for r in range(dil) for j in range(nkt)]
                                if hh == 0 or True:
                                    build_va(va, ("VA", vi), toks, vT, vkeys)
                                qbs = []
                                for r in range(dil):
                                    for i in range(nkt + 1):
                                        lo_l = 128 * i - 64
                                        q0 = max(lo_l, 0)
                                        q1 = min(lo_l + 128, L)
                                        n = q1 - q0
                                        qo = q0 - lo_l
                                        items = []
                                        if i >= 1:
                                            j = i - 1
                                            ksl = ssl(dil * 128 * j + r, 128, dil)
                                            items.append((kT[hs, ksl], va[:, r * nkt + j, hh, :],
                                                          biasA[:, hh, p, qo:qo + n]))
                                        if i <= nkt - 1:
                                            j = i
                                            ksl = ssl(dil * 128 * j + r, 128, dil)
                                            items.append((kT[hs, ksl], va[:, r * nkt + j, hh, :],
                                                          biasA[:, hh, p, 128 + qo:128 + qo + n]))
                                        qsl = ssl(dil * q0 + r, n, dil)
                                        qbs.append(dict(q=qT[hs, qsl], n=n, items=items, dst=acc[:, qsl],
                                                        add=(p > 0)))
                                attention(qbs, qkeys, kkeys, [("VA", vi), ("VA", vi, "b")], ["biasA"], "acc")
                            head_epilogue(hh, pr, "acc")
                    elif grp_name == "B":
                        vi = nxt("va")
                        va = VA[vi]
                        toks = [slice(128 * j, 128 * j + 128) for j in range(NT)]
                        build_va(va, ("VA", vi), toks, vT, vkeys)
                        for hh in range(2):
                            hs = slice(64 * hh, 64 * hh + 64)
                            dma("pool", "biasB", [(biasB, bias_b_d[l, 2 * pr + hh])], writes=["biasB"])
                            qbs = []
                            for T in range(NT):
                                items = []
                                for J in na_keytiles(T):
                                    v = na_variant(T, J)
                                    items.append((kT[hs, 128 * J:128 * J + 128], va[:, J, hh, :], biasB[:, v, :]))
                                qsl = slice(128 * T, 128 * T + 128)
                                qbs.append(dict(q=qT[hs, qsl], n=128, items=items[:4], dst=acc[:, qsl], add=False))
                                if len(items) > 4:
                                    qbs.append(dict(q=qT[hs, qsl], n=128, items=items[4:], dst=acc[:, qsl], add=True))
                            attention(qbs, qkeys, kkeys, [("VA", vi), ("VA", vi, "b")], ["biasB"], "acc")
                            head_epilogue(hh, pr, "acc")
                    else:
                        for hh in range(2):
                            hs = slice(64 * hh, 64 * hh + 64)
                            qbs = []
                            for b in range(4):
                                qsl = slice(512 * b, 512 * b + 512)
                                items = [(kmT[hs, pr, 128 * j:128 * j + 128], VAM[:, pr, j, hh, :], None) for j in range(2)]
                                qbs.append(dict(q=qT[hs, qsl], n=512, items=items[:1], dst=acc[:, qsl], add=False))
                                qbs.append(dict(q=qT[hs, qsl], n=512, items=items[1:], dst=acc[:, qsl], add=True))
                            attention(qbs, qkeys, [("kmT", pr)], ["VAM", ("VAM", "b")], [], "acc")
                            head_epilogue(hh, pr, "acc")
                if stop == grp_name + "attn":
                    return
                if grp_name == "A":
                    group_finalize(l, [0, 1, 2], 384)
                elif grp_name == "B":
                    group_finalize(l, [3, 4, 5], 384)
                else:
                    group_finalize(l, [6, 7], 256)
                if stop == grp_name:
                    return

        def mem_path(s, l):
            dma("sp", "mem", [(mem_sb, mem_d[s].rearrange("(t p) d -> p t d", p=128))],
                reads=[], writes=[("mem", 0), ("mem", 1), "acc"])
            norm_to_hT(mem_sb, "mem", 2, g_mem_d[l:l + 1, :], hT, "hT")
            if stop and stop.startswith("n"):
                return
            rd = {}
            for t in range(2):
                for k, v in sc.res[("mem", t)]["r"].items():
                    if k not in rd or rd[k][2] < v[2]:
                        rd[k] = v
            sc.res["acc"] = dict(w=None, r=rd)
            for j in range(4):
                if stop == "m0" or (stop == "m1" and j >= 1) or (stop == "m2" and j >= 2) or (stop == "m3" and j >= 3):
                    return
                wi = nxt("wt", len(Wt))
                dma("pool", "Wt%d" % wi, [(Wt[wi][:, 0, :, :], w_mem_d[l, j])], writes=[("Wt", wi)])
                if j < 2:
                    def consume(bank, bkey, b, n, j=j):
                        qk_consume(kmT[:, j, :], ("kmTb", j), qkg[:, l * 6 + 5:l * 6 + 6])(bank, bkey, b, n)
                        sc.res[("kmT", j)] = sc.res[(("kmTb", j), 0)]
                    proj_chunk(lambda kc, wi=wi: Wt[wi][:, 0, kc, :], ("Wt", wi), hT, "hT", MEM, consume)
                else:
                    proj_chunk(lambda kc, wi=wi: Wt[wi][:, 0, kc, :], ("Wt", wi), hT, "hT", MEM,
                               copy_consume(vmT, "vmT"))
                    pr = j - 2
                    toks = [slice(0, 128), slice(128, 256)]

                    def tr(e):
                        ins = None
                        for t in range(2):
                            ins = e.transpose(pTr[:, t, :], vmT[:, toks[t]], ident[:])
                        return ins
                    op("pe", tr, reads=[("vmT", 0), "ident"], writes=[BK_TR])
                    op("dve", lambda e, pr=pr: e.tensor_copy(VAM[:, pr, :, 0, 0:64], pTr[:, 0:2, 0:64]),
                       reads=[BK_TR], writes=["VAM"])
                    op("dve", lambda e, pr=pr: e.tensor_copy(VAM[:, pr, :, 1, 64:128], pTr[:, 0:2, 64:128]),
                       reads=[BK_TR], writes=[("VAM", "b")])

        def moe_phase(s, l):
            norm_to_hT(x_sb, "x", NT, g_ffn_d[l:l + 1, :], hT, "hT")
            dma("pool", "Wr", [(Wr[:], w_rt_d[l])], writes=["Wr"])
            dma("sp", "brt", [(brt[:], b_rt_d[l:l + 1, :].partition_broadcast(128))], writes=["brt"])
            for t in range(NT):
                bank, bkey = B[2]

                def mm(e, t=t):
                    ins = None
                    for kc in range(8):
                        ins = e.matmul(bank[:, 0:36], hT[:, kc, t * 128:(t + 1) * 128], Wr[:, kc, :],
                                       start=(kc == 0), stop=(kc == 7))
                    return ins
                op("pe", mm, reads=[("hT", t), "Wr"], writes=[bkey])
                op("dve", lambda e: e.tensor_tensor(lg[:], bank[:, 0:36], brt[:], ALU.add),
                   reads=[bkey, "brt"], writes=["lg"])
                op("dve", lambda e: e.reduce_max(rt_s[:, 0:1], lg[:, 0:4], AX.X), reads=["lg"], writes=["rt0"])
                op("dve", lambda e: e.tensor_scalar_mul(rt_s[:, 1:2], rt_s[:, 0:1], -1.0), reads=["rt0"], writes=["rt1"])
                op("act", lambda e: e.activation(junk[:, 0:4], lg[:, 0:4], AF.Exp, bias=rt_s[:, 1:2],
                                                 accum_out=rt_s[:, 2:3]),
                   reads=["lg", "rt1"], writes=["junk", "rt2"])
                op("dve", lambda e: e.reciprocal(rt_s[:, 3:4], rt_s[:, 2:3]), reads=["rt2"], writes=["rt3"])
                op("dve", lambda e: e.tensor_scalar(rt_s[:, 4:8], lg[:, 0:4], rt_s[:, 0:1], NEG * 10, ALU.is_lt, ALU.mult),
                   reads=["lg", "rt0"], writes=["rt4"])
                for g in range(4):
                    op("dve", lambda e, g=g: e.tensor_scalar_add(lmask[:, g * 8:(g + 1) * 8], lg[:, 4 + g * 8:12 + g * 8],
                                                                 rt_s[:, 4 + g:5 + g]),
                       reads=["lg", "rt4"], writes=[("lmask", g)])
                lm = [("lmask", g) for g in range(4)]
                op("dve", lambda e: e.reduce_max(mx8[:, 0:1], lmask[:], AX.X), reads=lm, writes=["mx8a"])
                op("dve", lambda e: e.tensor_scalar(eq2[:], lmask[:], mx8[:, 0:1], NEG * 10, ALU.is_equal, ALU.mult),
                   reads=lm + ["mx8a"], writes=["eq2"])
                op("dve", lambda e: e.tensor_tensor(eq1[:], lmask[:], eq2[:], ALU.add), reads=lm + ["eq2"], writes=["eq1"])
                op("dve", lambda e: e.reduce_max(mx8[:, 1:2], eq1[:], AX.X), reads=["eq1"], writes=["mx8"])
                op("dve", lambda e: e.tensor_tensor(rt_s[:, 8:9], mx8[:, 1:2], mx8[:, 0:1], ALU.subtract),
                   reads=["mx8", "mx8a"], writes=["rt8"])
                op("act", lambda e: e.activation(rt_s[:, 9:10], rt_s[:, 8:9], AF.Exp), reads=["rt8"], writes=["rt9"])
                op("dve", lambda e: e.tensor_scalar_add(rt_s[:, 9:10], rt_s[:, 9:10], 1.0), reads=["rt9"], writes=["rt9"])
                op("dve", lambda e: e.reciprocal(rt_s[:, 12:13], rt_s[:, 9:10]), reads=["rt9"], writes=["rt12"])
                op("dve", lambda e: e.tensor_tensor(rt_s[:, 10:11], rt_s[:, 3:4], rt_s[:, 12:13], ALU.mult),
                   reads=["rt12", "rt3"], writes=["rt10"])
                op("dve", lambda e: e.tensor_tensor(rt_s[:, 11:12], rt_s[:, 3:4], rt_s[:, 10:11], ALU.subtract),
                   reads=["rt10", "rt3"], writes=["rt11"])
                op("dve", lambda e: e.tensor_scalar(eq1[:], lmask[:], mx8[:, 0:1], rt_s[:, 10:11], ALU.is_equal, ALU.mult),
                   reads=lm + ["mx8", "mx8a", "rt10"], writes=["eq1"])
                op("dve", lambda e: e.tensor_scalar(eq2[:], lmask[:], mx8[:, 1:2], rt_s[:, 11:12], ALU.is_equal, ALU.mult),
                   reads=lm + ["mx8", "rt11"], writes=["eq2"])
                op("dve", lambda e, t=t: e.tensor_tensor(gates[:, t, :], eq1[:], eq2[:], ALU.add),
                   reads=["eq1", "eq2"], writes=[("gates", t)])
            def load_e(e_):
                wi = nxt("wm")
                dma("pool", "We%d" % wi, [(Wg[wi][:], w_gate_d[l, e_]), (Wu[wi][:], w_up_d[l, e_]),
                                          (Wd[wi][:], w_down_d[l, e_])], writes=[("We", wi)])
                return wi
            wi_next = load_e(0)
            for e_ in range(n_experts):
                wi = wi_next
                if e_ + 1 < n_experts:
                    wi_next = load_e(e_ + 1)
                for b in range(4):
                    hi_ = nxt("hid")
                    for dc in range(4):
                        gb = nxt("proj")
                        gbank, gkey = B[gb]
                        ubank, ukey = B[2] if False else (None, None)
                        ui = nxt("st")
                        ubank, ukey = (pY[0][:, ui * 512:(ui + 1) * 512], Yk[0][ui])

                        def mmg(e, wi=wi, b=b, dc=dc, gbank=gbank):
                            ins = None
                            for kc in range(8):
                                ins = e.matmul(gbank[:], Wg[wi][:, kc, dc * 128:(dc + 1) * 128], hT[:, kc, b * 512:(b + 1) * 512],
                                               start=(kc == 0), stop=(kc == 7))
                            return ins

                        def mmu(e, wi=wi, b=b, dc=dc, ubank=ubank):
                            ins = None
                            for kc in range(8):
                                ins = e.matmul(ubank, Wu[wi][:, kc, dc * 128:(dc + 1) * 128], hT[:, kc, b * 512:(b + 1) * 512],
                                               start=(kc == 0), stop=(kc == 7))
                            return ins
                        hk = [("hT", t) for t in range(4 * b, 4 * b + 4)]
                        op("pe", mmg, reads=[("We", wi)] + hk, writes=[gkey])
                        op("pe", mmu, reads=[("We", wi)] + hk, writes=[ukey])
                        si = nxt("sg")
                        op("act", lambda e, si=si, gbank=gbank: e.activation(sg[si][:], gbank[:], AF.Silu),
                           reads=[gkey], writes=[("sg", si)])
                        op("dve", lambda e, si=si, ubank=ubank, hi_=hi_, dc=dc: e.tensor_tensor(
                            hid[hi_][:, dc, :], ubank, sg[si][:], ALU.mult),
                           reads=[ukey, ("sg", si)], writes=[("hid", hi_, dc)])
                    for tt in range(4):
                        t = 4 * b + tt
                        yi = 1

                        def mmd(e, wi=wi, hi_=hi_, tt=tt):
                            ins = None
                            for half in range(2):
                                for dc in range(4):
                                    ins = e.matmul(pY[1][:, half * 512:(half + 1) * 512], hid[hi_][:, dc, tt * 128:(tt + 1) * 128],
                                                   Wd[wi][:, dc, half * 512:(half + 1) * 512], start=(dc == 0), stop=(dc == 3))
                            return ins
                        op("pe", mmd, reads=[("We", wi)] + [("hid", hi_, dc) for dc in range(4)], writes=list(Yk[1]))
                        op("dve", lambda e, t=t, e_=e_: e.scalar_tensor_tensor(
                            x_sb[:, t, :], pY[1][:], gates[:, t, e_:e_ + 1], x_sb[:, t, :], ALU.mult, ALU.add),
                           reads=list(Yk[1]) + [("x", t), ("gates", t)], writes=[("x", t)])

        def moe_phase_sparse(s, l):
            dma("pool", "Wr", [(Wr[:], w_rt_d[l])], writes=["Wr"])
            dma("sp", "brt", [(brt[:], b_rt_d[l:l + 1, :].partition_broadcast(128))], writes=["brt"])
            dma("sp", "gbc", [(gbc[:], g_ffn_d[l:l + 1, :].partition_broadcast(128))], writes=["gbc"])

            def load_e(e_):
                wi = nxt("wm")
                dma("pool", "We%d" % wi, [(Wg[wi][:], w_gate_d[l, e_]), (Wu[wi][:], w_up_d[l, e_]),
                                          (Wd[wi][:], w_down_d[l, e_])], writes=[("We", wi)])
                return wi
            wi_next = load_e(0)
            scat_keys = []
            for t in range(NT):
                op("act", lambda e, t=t: e.activation(junk[:], x_sb[:, t, :], AF.Square, accum_out=ssum[:, t:t + 1]),
                   reads=[("x", t)], writes=["junk", ("ssum", t)])
                op("act", lambda e, t=t: e.activation(rstd[:, t:t + 1], ssum[:, t:t + 1], AF.Ln, bias=float(EPS), scale=1.0 / D),
                   reads=[("ssum", t)], writes=[("rstdq", t), ("rstd", t)])
                op("act", lambda e, t=t: e.activation(rstd[:, t:t + 1], rstd[:, t:t + 1], AF.Exp, scale=-0.5),
                   reads=[("rstdq", t)], writes=[("rstd", t)])
                hb = nxt("ht")
                op("dve", lambda e, t=t, hb=hb: e.scalar_tensor_tensor(htok[hb][:], x_sb[:, t, :], rstd[:, t:t + 1], gbc[:], ALU.mult, ALU.mult),
                   reads=[("x", t), ("rstd", t), "gbc"], writes=[("htok", hb)])

                def tr(e, hb=hb):
                    ins = None
                    for c in range(8):
                        ins = e.transpose(pTr[:, c, :], htok[hb][:, c * 128:(c + 1) * 128], ident[:])
                    return ins
                op("pe", tr, reads=[("htok", hb), "ident"], writes=[BK_TR])
                op("act", lambda e, t=t: e.copy(hT[:, :, t * 128:(t + 1) * 128], pTr[:]), reads=[BK_TR], writes=[("hT", t)])
                bank, bkey = B[2]

                def mm(e, t=t):
                    ins = None
                    for kc in range(8):
                        ins = e.matmul(bank[:, 0:36], hT[:, kc, t * 128:(t + 1) * 128], Wr[:, kc, :], start=(kc == 0), stop=(kc == 7))
                    return ins
                op("pe", mm, reads=[("hT", t), "Wr"], writes=[bkey])
                op("dve", lambda e: e.tensor_tensor(lg[:], bank[:, 0:36], brt[:], ALU.add), reads=[bkey, "brt"], writes=["lg"])
                op("dve", lambda e: e.reduce_max(rt_s[:, 0:1], lg[:, 0:4], AX.X), reads=["lg"], writes=["rt0"])
                op("dve", lambda e: e.tensor_scalar_mul(rt_s[:, 1:2], rt_s[:, 0:1], -1.0), reads=["rt0"], writes=["rt1"])
                op("act", lambda e: e.activation(junk[:, 0:4], lg[:, 0:4], AF.Exp, bias=rt_s[:, 1:2], accum_out=rt_s[:, 2:3]),
                   reads=["lg", "rt1"], writes=["junk", "rt2"])
                op("dve", lambda e: e.reciprocal(rt_s[:, 3:4], rt_s[:, 2:3]), reads=["rt2"], writes=["rt3"])
                op("dve", lambda e: e.tensor_scalar(rt_s[:, 4:8], lg[:, 0:4], rt_s[:, 0:1], NEG * 10, ALU.is_lt, ALU.mult),
                   reads=["lg", "rt0"], writes=["rt4"])
                for g in range(4):
                    op("dve", lambda e, g=g: e.tensor_scalar_add(lmask[:, g * 8:(g + 1) * 8], lg[:, 4 + g * 8:12 + g * 8], rt_s[:, 4 + g:5 + g]),
                       reads=["lg", "rt4"], writes=[("lmask", g)])
                lm = [("lmask", g) for g in range(4)]
                op("dve", lambda e: e.reduce_max(mx8[:, 0:1], lmask[:], AX.X), reads=lm, writes=["mx8a"])
                op("dve", lambda e: e.tensor_scalar(eq1[:], lmask[:], mx8[:, 0:1], None, ALU.is_equal), reads=lm + ["mx8a"], writes=["eq1"])
                op("dve", lambda e: e.scalar_tensor_tensor(lmask[:], eq1[:], NEG * 10, lmask[:], ALU.mult, ALU.add),
                   reads=lm + ["eq1"], writes=lm)
                op("dve", lambda e: e.reduce_max(mx8[:, 1:2], lmask[:], AX.X), reads=lm, writes=["mx8"])
                op("dve", lambda e: e.tensor_scalar(eq2[:], lmask[:], mx8[:, 1:2], None, ALU.is_equal), reads=lm + ["mx8"], writes=["eq2"])
                op("dve", lambda e: e.tensor_tensor(rt_s[:, 8:9], mx8[:, 1:2], mx8[:, 0:1], ALU.subtract), reads=["mx8", "mx8a"], writes=["rt8"])
                op("act", lambda e: e.activation(rt_s[:, 9:10], rt_s[:, 8:9], AF.Exp), reads=["rt8"], writes=["rt9"])
                op("dve", lambda e: e.tensor_scalar_add(rt_s[:, 9:10], rt_s[:, 9:10], 1.0), reads=["rt9"], writes=["rt9"])
                op("dve", lambda e: e.reciprocal(rt_s[:, 12:13], rt_s[:, 9:10]), reads=["rt9"], writes=["rt12"])
                op("dve", lambda e, t=t: e.tensor_tensor(g01[:, t, 0:1], rt_s[:, 3:4], rt_s[:, 12:13], ALU.mult),
                   reads=["rt12", "rt3"], writes=[("g0", t)])
                op("dve", lambda e, t=t: e.tensor_tensor(g01[:, t, 1:2], rt_s[:, 3:4], g01[:, t, 0:1], ALU.subtract),
                   reads=[("g0", t), "rt3"], writes=[("g1", t)])
                op("dve", lambda e, t=t: e.tensor_tensor(Mall[:, t, :], eq1[:], eq2[:], ALU.add), reads=["eq1", "eq2"], writes=[("M", t)])
                def rk(e, t=t):
                    ins = None
                    for tp in range(t):
                        ins = e.matmul(bank[:, 64:64 + NE], ones[:], Mall[:, tp, :], start=(tp == 0), stop=False)
                    ins = e.matmul(bank[:, 64:64 + NE], triu[:], Mall[:, t, :], start=(t == 0), stop=True)
                    return ins
                op("pe", rk, reads=[("M", tp) for tp in range(t + 1)] + ["ones", "triu", "lg"], writes=[bkey])
                op("dve", lambda e: e.tensor_tensor(slotf[:], bank[:, 64:64 + NE], eoff[:], ALU.add), reads=[bkey, "eoff"], writes=["slotf"])
                op("dve", lambda e: e.tensor_tensor(eq1[:], eq1[:], slotf[:], ALU.mult), reads=["eq1", "slotf", ("M", t)], writes=["eq1"])
                op("dve", lambda e: e.tensor_tensor(eq2[:], eq2[:], slotf[:], ALU.mult), reads=["eq2", "slotf", ("M", t)], writes=["eq2"])
                op("dve", lambda e: e.reduce_sum(slot01f[:, 0:1], eq1[:], AX.X), reads=["eq1"], writes=["s0f"])
                op("dve", lambda e: e.reduce_sum(slot01f[:, 1:2], eq2[:], AX.X), reads=["eq2"], writes=["s1f"])
                op("dve", lambda e, t=t: e.tensor_copy(slot01[:, t, :], slot01f[:]), reads=["s0f", "s1f"], writes=[("slot", t)])
                for k in range(2):
                    def sc_fn(e, sem, t=t, k=k, hb=hb):
                        e.indirect_dma_start(out=Hs_d[:, :], out_offset=bass.IndirectOffsetOnAxis(ap=slot01[:, t, k:k + 1], axis=0),
                                             in_=htok[hb][:, :], in_offset=None).then_inc(sem, 16)
                    op("pool", sc_fn, reads=[("slot", t), ("htok", hb)], writes=[("Hs", t, k)], dma_key="hs", ndma=1)
                    scat_keys.append(("Hs", t, k))
            bank, bkey = B[2]

            def cntmm(e):
                ins = None
                for tp in range(NT):
                    ins = e.matmul(bank[:, 128:128 + NE], ones[:], Mall[:, tp, :], start=(tp == 0), stop=(tp == NT - 1))
                return ins
            op("pe", cntmm, reads=[("M", tp) for tp in range(NT)] + ["ones", "slotf"], writes=[bkey])
            op("dve", lambda e: e.tensor_scalar(cntf[:, 0:NE], bank[:, 128:128 + NE], 256.5, None, ALU.is_gt), reads=[bkey], writes=["cntf"])
            op("dve", lambda e: e.reduce_max(cntf[:, NE:NE + 1], cntf[:, 0:NE], AX.X), reads=["cntf"], writes=["cntf2"])
            op("dve", lambda e: e.tensor_copy(flag_i[:], cntf[:]), reads=["cntf", "cntf2"], writes=["flag"])

            ystore_keys = []

            def prefetch_hs(e_, j0):
                r0 = e_ * S + j0 * 128
                hi = e_ % 2
                dma("sp", "hsl%d" % hi, [(hs_tok[hi], Hs_d[r0:r0 + 256, :].rearrange("(j p) d -> p j d", p=128))],
                    reads=scat_keys, writes=[("hst", hi)])
                return hi

            def expert_tiles(e_, wi, j0, hi=None):
                r0 = e_ * S + j0 * 128
                if hi is None:
                    hi = prefetch_hs(e_, j0)
                he = nxt("hte")
                for j in range(2):
                    def tr(e, j=j, hi=hi):
                        ins = None
                        for c in range(8):
                            ins = e.transpose(pTr[:, c, :], hs_tok[hi][:, j, c * 128:(c + 1) * 128], ident[:])
                        return ins
                    op("pe", tr, reads=[("hst", hi), "ident"], writes=[BK_TR])
                    op("act", lambda e, j=j, he=he: e.copy(hTe[he][:, :, j * 128:(j + 1) * 128], pTr[:]),
                       reads=[BK_TR], writes=[("hte", he, j)])
                hk = [("hte", he, 0), ("hte", he, 1)]
                hi_ = nxt("hid")
                for dc in range(4):
                    gb = nxt("proj")
                    gbank, gkey = B[gb]
                    ui = nxt("st")
                    ubank, ukey = (pY[0][:, ui * 512:ui * 512 + 256], Yk[0][ui])

                    def mmg(e, dc=dc, gbank=gbank):
                        ins = None
                        for kc in range(8):
                            ins = e.matmul(gbank[:, 0:256], Wg[wi][:, kc, dc * 128:(dc + 1) * 128], hTe[he][:, kc, :],
                                           start=(kc == 0), stop=(kc == 7))
                        return ins

                    def mmu(e, dc=dc, ubank=ubank):
                        ins = None
                        for kc in range(8):
                            ins = e.matmul(ubank, Wu[wi][:, kc, dc * 128:(dc + 1) * 128], hTe[he][:, kc, :],
                                           start=(kc == 0), stop=(kc == 7))
                        return ins
                    op("pe", mmg, reads=[("We", wi)] + hk, writes=[gkey])
                    op("pe", mmu, reads=[("We", wi)] + hk, writes=[ukey])
                    si = nxt("sg")
                    op("act", lambda e, si=si, gbank=gbank: e.activation(sg[si][:, 0:256], gbank[:, 0:256], AF.Silu),
                       reads=[gkey], writes=[("sg", si)])
                    op("dve", lambda e, si=si, ubank=ubank, dc=dc: e.tensor_tensor(hid[hi_][:, dc, 0:256], ubank, sg[si][:, 0:256], ALU.mult),
                       reads=[ukey, ("sg", si)], writes=[("hid", hi_, dc)])
                for j in range(2):
                    def mmd(e, j=j):
                        ins = None
                        for half in range(2):
                            for dc in range(4):
                                ins = e.matmul(pY[1][:, half * 512:(half + 1) * 512], hid[hi_][:, dc, j * 128:(j + 1) * 128],
                                               Wd[wi][:, dc, half * 512:(half + 1) * 512], start=(dc == 0), stop=(dc == 3))
                        return ins
                    op("pe", mmd, reads=[("We", wi)] + [("hid", hi_, dc) for dc in range(4)], writes=list(Yk[1]))
                    yo = nxt("yo")
                    if j == 0:
                        op("act", lambda e, yo=yo: e.copy(yout[yo], pY[1][:]), reads=list(Yk[1]), writes=[("yo", yo)])
                    else:
                        op("dve", lambda e, yo=yo: e.tensor_copy(yout[yo], pY[1][:]), reads=list(Yk[1]), writes=[("yo", yo)])
                    rr0 = r0 + j * 128
                    dma("sp", "ys", [(Ys_d[rr0:rr0 + 128, :], yout[yo])], reads=[("yo", yo)], writes=[("Ys", e_, j0 + j)])
                    ystore_keys.append(("Ys", e_, j0 + j))

            hi_next = prefetch_hs(0, 0)
            for e_ in range(n_experts):
                wi = wi_next
                hi_cur = hi_next
                if e_ + 1 < n_experts:
                    wi_next = load_e(e_ + 1)
                    hi_next = prefetch_hs(e_ + 1, 0)
                expert_tiles(e_, wi, 0, hi_cur)
            if guard:
                sc.begin_guard(flag_i[0:1, NE:NE + 1], "flag")
                wi_next = load_e(0)
                for e_ in range(n_experts):
                    wi = wi_next
                    if e_ + 1 < n_experts:
                        wi_next = load_e(e_ + 1)
                    for j0 in range(2, NT, 2):
                        expert_tiles(e_, wi, j0)
                sc.end_guard()
            ykeys = list(dict.fromkeys(ystore_keys))
            for t in range(NT):
                for k in range(2):
                    yg = nxt("yg")

                    def g_fn(e, sem, t=t, k=k, yg=yg):
                        e.indirect_dma_start(out=Yg[yg][:, :], out_offset=None, in_=Ys_d[:, :],
                                             in_offset=bass.IndirectOffsetOnAxis(ap=slot01[:, t, k:k + 1], axis=0)).then_inc(sem, 16)
                    op("pool", g_fn, reads=ykeys + [("slot", t)], writes=[("yg", yg)], dma_key="yg%d" % yg, ndma=1)
                    op("dve", lambda e, t=t, k=k, yg=yg: e.scalar_tensor_tensor(x_sb[:, t, :], Yg[yg], g01[:, t, k:k + 1], x_sb[:, t, :],
                                                                                ALU.mult, ALU.add),
                       reads=[("yg", yg), ("x", t), ("g0", t), ("g1", t)], writes=[("x", t)])

        def barrier():
            evs = []
            for name, E in sc.eng.items():
                if E["count"] > 0:
                    evs.append((name, E["sem"], E["count"], name))
            for k, d in sc.dsem.items():
                evs.append(("d_" + str(k), d[0], d[1], "dma"))
            for name in ("pe", "act", "dve", "pool", "sp"):
                sc.wait_all(name, [ev for ev in evs if ev[0] != name])

        store_events = []
        for s in range(nseq):
            dma("sp", "xin", [(x_sb[:, 4 * g:4 * g + 4, :], x_d[s, 512 * g:512 * g + 512, :].rearrange("(t p) d -> p t d", p=128))
                              for g in range(4)],
                writes=[("x", t) for t in range(NT)])
            for l in range(depth):
                if do_attn:
                    barrier()
                    attn_phase(s, l)
                if do_moe:
                    barrier()
                    if sparse:
                        moe_phase_sparse(s, l)
                    else:
                        moe_phase(s, l)
            ev = dma("sp", "xout", [(y_d[s, 512 * g:512 * g + 512, :].rearrange("(t p) d -> p t d", p=128), x_sb[:, 4 * g:4 * g + 4, :])
                                    for g in range(4)],
                     reads=[("x", t) for t in range(NT)])
            store_events.append(ev)
        sc.wait_all("sp", store_events)
        sc.emit()
    nc._in_names = list(dr)
    return nc


_CACHE = {}


def kernel(**inputs):
    shared = _host_prep(inputs)
    x = np.ascontiguousarray(np.asarray(inputs["x"], dtype=np.float32))
    mem = np.ascontiguousarray(np.asarray(inputs["mem"], dtype=np.float32))
    if "nc" not in _CACHE:
        _CACHE["nc"] = build_nc()
    nc = _CACHE["nc"]
    in_maps = []
    for c in range(N_CORES):
        m = dict(shared)
        m["x"] = x[c * NSEQ:(c + 1) * NSEQ]
        m["mem"] = mem[c * NSEQ:(c + 1) * NSEQ]
        in_maps.append(m)
    res = run_bass_kernel_spmd(nc, in_maps, core_ids=list(range(N_CORES)))
    out = np.concatenate([np.asarray(r["y"]) for r in res.results], axis=0)
    return out.astype(np.float32)
```

```python
import contextlib
import numpy as np
import concourse.bass as bass
import concourse.mybir as mybir
from concourse.bass_utils import run_bass_kernel_spmd

F32 = mybir.dt.float32
BF16 = mybir.dt.bfloat16
AF = mybir.ActivationFunctionType
ALU = mybir.AluOpType
AX = mybir.AxisListType

D = 1024
S = 2048
NT = 16
MEM = 256
DEPTH = 2
NSEQ = 4
NE = 32
DE = 512
EPS = 1e-6
NEG = -30000.0
N_CORES = 8


class Sched:
    def __init__(self, nc, stack):
        self.nc = nc
        self.stack = stack
        self.eng = {}
        for name in ("pe", "act", "dve", "pool", "sp"):
            sem = stack.enter_context(nc.semaphore("s_" + name))
            self.eng[name] = dict(sem=sem, count=0, ops=[], seen={})
        self.res = {}
        self.dsem = {}
        self.guard = None
        self.gopened = set()

    def _need(self, eng, reads, writes):
        need = {}

        def add(ev, kind):
            sid, sem, val, src = ev
            if src == eng:
                if eng in ("pe", "sp"):
                    return
                if kind != "raw":
                    return
            if need.get(sid, (None, 0))[1] < val:
                need[sid] = (sem, val)

        for k in reads:
            r = self.res.get(k)
            if r and r["w"]:
                add(r["w"], "raw")
        for k in writes:
            r = self.res.get(k)
            if r:
                if r["w"]:
                    add(r["w"], "waw")
                for e in r["r"].values():
                    add(e, "war")
        return need

    def begin_guard(self, flag_ap, flag_key):
        self.guard = (flag_ap, flag_key)
        self.gopened = set()
        self.gsnap = {}

    def end_guard(self):
        for eng in self.gopened:
            self.eng[eng]["ops"].append(("gclose",))
            self.eng[eng]["seen"] = self.gsnap[eng]
        self.guard = None
        self.gopened = set()

    def op(self, eng, fn, reads=(), writes=(), dma_key=None, ndma=0):
        E = self.eng[eng]
        first_guarded = False
        if self.guard is not None and eng not in self.gopened:
            fneed = self._need(eng, [self.guard[1]], [])
            fw = []
            for sid, (sem, val) in fneed.items():
                if E["seen"].get(sid, 0) >= val:
                    continue
                E["seen"][sid] = val
                fw.append((sem, val))
            drain = [(E["sem"], E["count"])] if (E["count"] > 0 and eng != "sp") else []
            dtot = {k: (d[0], d[1]) for k, d in self.dsem.items()}
            E["ops"].append(("gopen", fw, self.guard[0], drain, dtot))
            self.gopened.add(eng)
            self.gsnap[eng] = dict(E["seen"])
            first_guarded = True
        need = self._need(eng, reads, writes)
        waits = []
        for sid, (sem, val) in need.items():
            if E["seen"].get(sid, 0) >= val:
                continue
            E["seen"][sid] = val
            waits.append((sem, val))
        if dma_key is None:
            E["count"] += 1
            ev = (eng, E["sem"], E["count"], eng)
            inc = E["sem"]
            rkey = eng
        else:
            d = self.dsem.get(dma_key)
            if d is None:
                sem = self.stack.enter_context(self.nc.semaphore("d_" + str(dma_key)))
                d = self.dsem[dma_key] = [sem, 0]
            d[1] += 16 * ndma
            ev = ("d_" + str(dma_key), d[0], d[1], "dma")
            inc = ("dma", d[0])
            rkey = "d_" + str(dma_key)
        E["ops"].append((waits, fn, inc, 16 * ndma))
        if first_guarded:
            self.res.setdefault(self.guard[1], dict(w=None, r={}))["r"]["g_" + eng] = ev
        for k in writes:
            self.res[k] = dict(w=ev, r={})
        for k in reads:
            self.res.setdefault(k, dict(w=None, r={}))["r"][rkey] = ev
        return ev

    def wait_all(self, eng, events):
        E = self.eng[eng]
        waits = []
        for sid, sem, val, src in events:
            if E["seen"].get(sid, 0) >= val:
                continue
            E["seen"][sid] = val
            waits.append((sem, val))
        E["ops"].append((waits, None, None, 0))

    def emit(self):
        nc = self.nc
        with nc.Block() as block:
            table = (("pe", block.tensor), ("act", block.scalar), ("dve", block.vector),
                     ("pool", block.gpsimd), ("sp", block.sync))
            for name, deco in table:
                ops = self.eng[name]["ops"]

                def body(e, ops=ops, name=name):
                    def real(rec):
                        waits, fn, inc, nd = rec
                        for sem, val in waits:
                            e.wait_ge(sem, val)
                        if fn is None:
                            return
                        if isinstance(inc, tuple):
                            fn(e, inc[1])
                        else:
                            fn(e).then_inc(inc, 1)

                    def ghost(rec):
                        waits, fn, inc, nd = rec
                        for sem, val in waits:
                            e.wait_ge(sem, val)
                        if fn is None:
                            return
                        if isinstance(inc, tuple):
                            e.sem_inc(inc[1], nd)
                        else:
                            e.sem_inc(inc, 1)

                    with e.register("gr_" + name) as greg:
                        i = 0
                        n = len(ops)
                        while i < n:
                            rec = ops[i]
                            if rec[0] == "gopen":
                                for sem, val in rec[1]:
                                    e.wait_ge(sem, val)
                                e.reg_load(greg, rec[2])
                                j = i + 1
                                blk = []
                                while ops[j][0] != "gclose":
                                    blk.append(ops[j])
                                    j += 1
                                with e.If_ne(greg, 0):
                                    for r in blk:
                                        real(r)
                                with e.Else():
                                    for sem, val in rec[3]:
                                        e.wait_ge(sem, val)
                                    nown = 0
                                    dadd = {}
                                    for r in blk:
                                        if r[1] is None:
                                            continue
                                        if isinstance(r[2], tuple):
                                            dadd[id(r[2][1])] = (r[2][1], dadd.get(id(r[2][1]), (None, 0))[1] + r[3])
                                        else:
                                            nown += 1
                                    for sem, add in dadd.values():
                                        for k, (dsem_h, tot) in rec[4].items():
                                            if dsem_h is sem and tot > 0:
                                                e.wait_ge(sem, tot)
                                        e.sem_inc(sem, add)
                                    if nown:
                                        e.sem_inc(self.eng[name]["sem"], nown)
                                i = j + 1
                                continue
                            real(rec)
                            i += 1

                deco(body)


def ssl(start, n, step):
    return slice(start, start + step * (n - 1) + 1, step)


def _alibi_slopes(n):
    return (2.0 ** (-8.0 * np.arange(1, n + 1) / n)).astype(np.float32)


def _bias_a():
    sl = _alibi_slopes(6)
    k = np.arange(128)[:, None]
    q = np.arange(128)[None, :]
    out = np.empty((6, 3, 128, 256), np.float32)
    for h in range(6):
        for p, dil in enumerate((1, 4, 16)):
            lo = np.where(k >= q, -sl[h] * dil * np.abs(k - q - 64).astype(np.float32), NEG)
            hi = np.where(k <= q, -sl[h] * dil * np.abs(k - q + 64).astype(np.float32), NEG)
            out[h, p, :, :128] = lo
            out[h, p, :, 128:] = hi
    return out


NA_VARIANTS = [(5, 5 + d) for d in (-2, -1, 0, 1, 2)] + \
              [(T, J) for T in (0, 1) for J in range(4)] + \
              [(T, J) for T in (14, 15) for J in range(12, 16)]


def na_variant(T, J):
    if 2 <= T <= 13:
        return J - T + 2
    if T < 2:
        return 5 + T * 4 + J
    return 13 + (T - 14) * 4 + (J - 12)


def na_keytiles(T):
    if T < 2:
        return list(range(4))
    if T > 13:
        return list(range(12, 16))
    return list(range(T - 2, T + 3))


def _bias_b(rpb):
    kl = np.arange(128)
    kr_off, kc = kl // 64, kl % 64
    qr_off, qc = kl // 64, kl % 64
    out = np.empty((DEPTH, 6, 128, len(NA_VARIANTS), 128), np.float32)
    for v, (T, J) in enumerate(NA_VARIANTS):
        r = (2 * T + qr_off)[None, :]
        keyrow = (2 * J + kr_off)[:, None]
        r0 = np.clip(r - 4, 0, 24)
        row_ok = (keyrow >= r0) & (keyrow < r0 + 8)
        c0 = np.clip(qc - 8, 0, 48)[None, :]
        col_ok = (kc[:, None] >= c0) & (kc[:, None] < c0 + 16)
        ok = row_ok & col_ok
        dr = np.clip(keyrow - r, -7, 7) + 7
        dc = np.clip(kc[:, None] - qc[None, :], -15, 15) + 15
        g = rpb[:, :, dr, dc]
        out[:, :, :, v, :] = np.where(ok[None, None], g, np.float32(NEG))
    return out


def _host_prep(inp):
    f = lambda a: np.ascontiguousarray(np.asarray(a, dtype=np.float32))
    w_in = f(inp["w_in"]).reshape(DEPTH, 8, 128, 20, 128).transpose(0, 3, 2, 1, 4)
    w_mem = f(inp["w_mem_kv"]).reshape(DEPTH, 8, 128, 4, 128).transpose(0, 3, 2, 1, 4)
    w_out = f(inp["w_out"]).reshape(DEPTH, 8, 128, 1024).transpose(0, 2, 1, 3)
    w_gate = f(inp["w_gate"]).reshape(DEPTH, NE, 8, 128, DE).transpose(0, 1, 3, 2, 4)
    w_up = f(inp["w_up"]).reshape(DEPTH, NE, 8, 128, DE).transpose(0, 1, 3, 2, 4)
    w_down = f(inp["w_down"]).reshape(DEPTH, NE, 4, 128, D).transpose(0, 1, 3, 2, 4)
    w_rt = np.concatenate([f(inp["w_group"]), f(inp["w_router"])], axis=2)
    w_rt = w_rt.reshape(DEPTH, 8, 128, 36).transpose(0, 2, 1, 3)
    b_rt = np.concatenate([f(inp["b_group"]), f(inp["b_router"])], axis=1)
    qkg = np.tile(f(inp["qk_gain"]), (1, 1, 2)).transpose(2, 0, 1).reshape(128, DEPTH * 6)
    og = f(inp["out_gain"]).reshape(DEPTH, 8, 128).transpose(2, 0, 1).reshape(128, DEPTH * 8)
    shared = dict(
        w_in=np.ascontiguousarray(w_in), w_mem=np.ascontiguousarray(w_mem),
        w_out=np.ascontiguousarray(w_out), w_gate=np.ascontiguousarray(w_gate),
        w_up=np.ascontiguousarray(w_up), w_down=np.ascontiguousarray(w_down),
        w_rt=np.ascontiguousarray(w_rt), b_rt=np.ascontiguousarray(b_rt),
        g_mix=f(inp["norm_mix"]), g_mem=f(inp["norm_mem"]), g_ffn=f(inp["norm_ffn"]),
        qkg=np.ascontiguousarray(qkg), og=np.ascontiguousarray(og),
        bias_a=_bias_a(), bias_b=_bias_b(f(inp["rpb"])),
        ident=np.eye(128, dtype=np.float32),
        triu=np.triu(np.ones((128, 128), np.float32), 1),
        eoff=np.tile((np.arange(NE, dtype=np.float32) * S)[None, :], (128, 1)),
        bones=np.kron(np.eye(2, dtype=np.float32), np.ones((64, 64), np.float32)),
    )
    return shared


def build_nc(nseq=NSEQ, depth=DEPTH, do_attn=True, do_moe=True, n_experts=NE, stop=None, sparse=True, guard=True):
    nc = bass.Bass("TRN2", target_bir_lowering=False)
    dr = {}

    def din(name, shape):
        dr[name] = nc.dram_tensor(name, list(shape), F32, kind="ExternalInput").ap()
        return dr[name]

    x_d = din("x", (nseq, S, D))
    mem_d = din("mem", (nseq, MEM, D))
    w_in_d = din("w_in", (DEPTH, 20, 128, 8, 128))
    w_mem_d = din("w_mem", (DEPTH, 4, 128, 8, 128))
    w_out_d = din("w_out", (DEPTH, 128, 8, 1024))
    if do_moe:
        w_gate_d = din("w_gate", (DEPTH, NE, 128, 8, DE))
        w_up_d = din("w_up", (DEPTH, NE, 128, 8, DE))
        w_down_d = din("w_down", (DEPTH, NE, 128, 4, D))
    w_rt_d = din("w_rt", (DEPTH, 128, 8, 36))
    b_rt_d = din("b_rt", (DEPTH, 36))
    g_mix_d = din("g_mix", (DEPTH, D))
    g_mem_d = din("g_mem", (DEPTH, D))
    g_ffn_d = din("g_ffn", (DEPTH, D))
    qkg_d = din("qkg", (128, DEPTH * 6))
    og_d = din("og", (128, DEPTH * 8))
    bias_a_d = din("bias_a", (6, 3, 128, 256))
    bias_b_d = din("bias_b", (DEPTH, 6, 128, 21, 128))
    ident_d = din("ident", (128, 128))
    triu_d = din("triu", (128, 128))
    eoff_d = din("eoff", (128, NE))
    I32 = mybir.dt.int32
    Hs_d = nc.dram_tensor("Hs_scr", [NE * S, D], BF16).ap()
    Ys_d = nc.dram_tensor("Ys_scr", [NE * S, D], F32).ap()
    bones_d = din("bones", (128, 128))
    y_d = nc.dram_tensor("y", [nseq, S, D], F32, kind="ExternalOutput").ap()

    stack = contextlib.ExitStack()
    with stack:
        def sb(name, shape, dt=F32):
            return stack.enter_context(nc.sbuf_tensor(name, list(shape), dt))

        UN = 43 * 1024
        U = sb("U", (128, UN), BF16)

        class Bump:
            def __init__(self):
                self.off = 0

            def alloc(self, shape, dt=F32):
                n = int(np.prod(shape[1:]))
                ne = n * 2 if dt == F32 else n
                ne = (ne + 1) // 2 * 2
                assert self.off + ne <= UN, ("union overflow", self.off, ne)
                v = U[:, self.off:self.off + ne]
                self.off += ne
                if dt == F32:
                    v = v.bitcast(F32)
                if len(shape) == 3:
                    v = v.rearrange("p (a b) -> p a b", a=shape[1])
                elif len(shape) == 4:
                    v = v.rearrange("p (a b c) -> p a b c", a=shape[1], b=shape[2])
                elif len(shape) == 5:
                    v = v.rearrange("p (a b c d) -> p a b c d", a=shape[1], b=shape[2], c=shape[3])
                return v

        x_sb = sb("x_sb", (128, NT, D))
        hT = sb("hT", (128, 8, S), BF16)
        sq = [sb("sq%d" % i, (128, 512), BF16) for i in range(2)]
        rs = [sb("rs%d" % i, (128, 512)) for i in range(2)]
        htok = [sb("htok%d" % i, (128, D), BF16) for i in range(2)]
        junk = sb("junk", (128, D), BF16)
        gbc = sb("gbc", (128, D))
        ssum = sb("ssum", (128, 32))
        rstd = sb("rstd", (128, 32))
        ident = sb("ident_sb", (128, 128), BF16)
        bones = sb("bones_sb", (128, 128), BF16)
        ones = sb("ones_sb", (128, 128), BF16)
        qkg = sb("qkg_sb", (128, DEPTH * 6))
        qkg8 = sb("qkg8_sb", (128, DEPTH * 6))
        og = sb("og_sb", (128, DEPTH * 8))
        Wr = sb("Wr", (128, 8, 36), BF16)
        brt = sb("brt", (128, 36))
        gates = sb("gates", (128, NT, NE))
        lg = sb("lg", (128, 36))
        rt_s = sb("rt_s", (128, 16))
        mx8 = sb("mx8", (128, 8))
        lmask = sb("lmask", (128, NE))
        eq1 = sb("eq1", (128, NE))
        eq2 = sb("eq2", (128, NE))
        triu = sb("triu_sb", (128, 128), BF16)
        eoff = sb("eoff_sb", (128, NE))
        Mall = sb("Mall", (128, NT, NE), BF16)
        slotf = sb("slotf", (128, NE))
        slot01f = sb("slot01f", (128, 2))
        slot01 = sb("slot01", (128, NT, 2), I32)
        g01 = sb("g01", (128, NT, 2))
        cntf = sb("cntf", (128, 2 * NE))
        flag_i = sb("flag_i", (128, 2 * NE), I32)
        ba = Bump()
        oT = ba.alloc((128, 3, S), BF16)
        acc = ba.alloc((128, S))
        qT = ba.alloc((128, S), BF16)
        kT = ba.alloc((128, S), BF16)
        vT = ba.alloc((128, S), BF16)
        VA = [ba.alloc((128, NT, 2, 128), BF16) for i in range(2)]
        Wt = [ba.alloc((128, 3, 8, 128), BF16) for i in range(1)]
        Wo = ba.alloc((128, 3, D), BF16)
        biasA = ba.alloc((128, 2, 3, 256), BF16)
        biasB = ba.alloc((128, 21, 128), BF16)
        PT = [ba.alloc((128, 512), BF16) for i in range(2)]
        mem_sb = acc.rearrange("p (a b) -> p a b", a=2)
        kmT = ba.alloc((128, 2, MEM), BF16)
        vmT = ba.alloc((128, MEM), BF16)
        VAM = ba.alloc((128, 2, 2, 2, 128), BF16)
        bm = Bump()
        Wg = [bm.alloc((128, 8, DE), BF16) for i in range(2)]
        Wu = [bm.alloc((128, 8, DE), BF16) for i in range(2)]
        Wd = [bm.alloc((128, 4, D), BF16) for i in range(2)]
        sg = [bm.alloc((128, 256 if sparse else 512), BF16) for i in range(2)]
        hid = [bm.alloc((128, 4, 256 if sparse else 512), BF16) for i in range(2)]
        if sparse:
            hTe = [bm.alloc((128, 8, 256), BF16) for i in range(2)]
            hs_tok = [bm.alloc((128, 2, D), BF16) for i in range(2)]
            yout = [bm.alloc((128, D)) for i in range(2)]
            Yg = [bm.alloc((128, D)) for i in range(2)]

        def ps(name, shape, dt=F32):
            return stack.enter_context(nc.psum_tensor(name, list(shape), dt))
        pA = [ps("pA%d" % i, (128, 512)) for i in range(3)]
        pTr = ps("pTr", (128, 8, 128), BF16)
        pY = [ps("pY%d" % i, (128, 1024)) for i in range(2)]
        B = [(pA[0], ("ps", 0)), (pA[1], ("ps", 1)), (pA[2], ("ps", 2))]
        BK_TR = ("ps", 3)
        Yk = [(("ps", 4), ("ps", 5)), (("ps", 6), ("ps", 7))]

        sc = Sched(nc, stack)
        op = sc.op

        def dma(queue, key, pairs, reads=(), writes=()):
            def fn(e, sem, pairs=pairs):
                for o, i in pairs:
                    e.dma_start(out=o, in_=i).then_inc(sem, 16)
            return op(queue, fn, reads=reads, writes=writes, dma_key=key, ndma=len(pairs))

        dma("pool", "cst", [(ident[:], ident_d), (bones[:], bones_d)], writes=["ident", "bones"])
        dma("sp", "cst2", [(qkg[:], qkg_d), (og[:], og_d), (eoff[:], eoff_d)], writes=["qkg", "og", "eoff"])
        dma("pool", "cst3", [(triu[:], triu_d)], writes=["triu"])
        op("pool", lambda e: e.memset(ones[:], 1.0), writes=["ones"])
        op("dve", lambda e: e.tensor_scalar_mul(qkg8[:], qkg[:], 0.125), reads=["qkg"], writes=["qkg8"])

        rr = dict(hte=0, hst=0, yo=0, yg=0, proj=0, st=0, pv=0, tr=0, y=0, pt=0, sq=0, rs=0, ht=0, wt=0, va=0, sg=0, hid=0, wm=0)

        def nxt(name, n=2):
            v = rr[name]
            rr[name] = (v + 1) % n
            return v

        def norm_to_hT(src, src_key, ntile, gain_d_row, dstT, dst_key):
            dma("sp", "gbc", [(gbc[:], gain_d_row.partition_broadcast(128))], writes=["gbc"])
            if stop == "n1":
                return
            for t in range(ntile):
                op("act", lambda e, t=t: e.activation(junk[:], src[:, t, :], AF.Square,
                                                      accum_out=ssum[:, t:t + 1]),
                   reads=[(src_key, t)], writes=["junk", ("ssum", t)])
                if stop == "n2":
                    continue
                op("act", lambda e, t=t: e.activation(rstd[:, t:t + 1], ssum[:, t:t + 1], AF.Ln,
                                                      bias=float(EPS), scale=1.0 / D),
                   reads=[("ssum", t)], writes=[("rstdq", t), ("rstd", t)])
                op("act", lambda e, t=t: e.activation(rstd[:, t:t + 1], rstd[:, t:t + 1], AF.Exp, scale=-0.5),
                   reads=[("rstdq", t)], writes=[("rstd", t)])
                if stop == "n3":
                    continue
                hb = nxt("ht")
                op("dve", lambda e, t=t, hb=hb: e.scalar_tensor_tensor(
                    htok[hb][:], src[:, t, :], rstd[:, t:t + 1], gbc[:], ALU.mult, ALU.mult),
                   reads=[(src_key, t), ("rstd", t), "gbc"], writes=[("htok", hb)])
                if stop == "n4":
                    continue

                def tr(e, hb=hb):
                    ins = None
                    for c in range(8):
                        ins = e.transpose(pTr[:, c, :], htok[hb][:, c * 128:(c + 1) * 128], ident[:])
                    return ins
                op("pe", tr, reads=[("htok", hb), "ident"], writes=[BK_TR])
                if stop == "n5":
                    continue
                op("act", lambda e, t=t: e.copy(dstT[:, :, t * 128:(t + 1) * 128], pTr[:]),
                   reads=[BK_TR], writes=[(dst_key, t)])

        def proj_chunk(W_ap, w_key, srcT, src_key, ntok, consume):
            nblk = (ntok + 511) // 512
            pend = None
            for b in range(nblk):
                n = min(512, ntok - b * 512)
                bi = nxt("proj")
                bank, bkey = B[bi]

                def mm(e, b=b, n=n, bank=bank):
                    ins = None
                    for kc in range(8):
                        ins = e.matmul(bank[:, 0:n], W_ap(kc), srcT[:, kc, b * 512:b * 512 + n],
                                       start=(kc == 0), stop=(kc == 7))
                    return ins
                tiles = [(src_key, t) for t in range(b * 4, b * 4 + (n + 127) // 128)]
                op("pe", mm, reads=[w_key] + tiles, writes=[bkey])
                if pend is not None:
                    consume(*pend)
                pend = (bank, bkey, b, n)
            if pend is not None:
                consume(*pend)

        def qk_consume(dst, dst_key, gcol_ap):
            def consume(bank, bkey, b, n):
                si = nxt("sq")
                op("act", lambda e: e.activation(sq[si][:, 0:n], bank[:, 0:n], AF.Square),
                   reads=[bkey], writes=[("sq", si)])
                sbank, skey = B[2]
                op("pe", lambda e: e.matmul(sbank[:, 0:n], bones[:], sq[si][:, 0:n], start=True, stop=True),
                   reads=[("sq", si), "bones"], writes=[skey])
                ri = nxt("rs")
                op("act", lambda e: e.activation(rs[ri][:, 0:n], sbank[:, 0:n], AF.Ln, bias=float(EPS), scale=1.0 / 64),
                   reads=[skey], writes=[("rsl", ri), ("rs", ri)])
                op("act", lambda e: e.activation(rs[ri][:, 0:n], rs[ri][:, 0:n], AF.Exp, scale=-0.5),
                   reads=[("rsl", ri)], writes=[("rs", ri)])
                op("dve", lambda e: e.scalar_tensor_tensor(dst[:, b * 512:b * 512 + n], bank[:, 0:n], gcol_ap,
                                                           rs[ri][:, 0:n], ALU.mult, ALU.mult),
                   reads=[bkey, ("rs", ri), "qkg", "qkg8"], writes=[(dst_key, b)])
            return consume

        def copy_consume(dst, dst_key):
            def consume(bank, bkey, b, n):
                op("act", lambda e: e.copy(dst[:, b * 512:b * 512 + n], bank[:, 0:n]),
                   reads=[bkey], writes=[(dst_key, b)])
            return consume

        def build_va(va, va_key, tile_tokens, src, src_keys):
            nt = len(tile_tokens)
            for g0 in range(0, nt, 8):
                g1 = min(nt, g0 + 8)

                def tr(e, g0=g0, g1=g1):
                    ins = None
                    for t in range(g0, g1):
                        ins = e.transpose(pTr[:, t - g0, :], src[:, tile_tokens[t]], ident[:])
                    return ins
                op("pe", tr, reads=list(src_keys) + ["ident"], writes=[BK_TR])
                op("dve", lambda e, g0=g0, g1=g1: e.tensor_copy(va[:, g0:g1, 0, 0:64], pTr[:, 0:g1 - g0, 0:64]),
                   reads=[BK_TR], writes=[va_key])
                op("dve", lambda e, g0=g0, g1=g1: e.tensor_copy(va[:, g0:g1, 1, 64:128], pTr[:, 0:g1 - g0, 64:128]),
                   reads=[BK_TR], writes=[va_key + ("b",)])

        def attention(qblocks, q_keys, k_keys, va_keys, bias_keys, acc_key):
            i = 0
            nq = len(qblocks)
            prev_stage2 = None
            while i < nq:
                cols = 0
                grp = []
                while i < nq and cols + qblocks[i]["n"] * len(qblocks[i]["items"]) <= 512:
                    grp.append(qblocks[i])
                    cols += qblocks[i]["n"] * len(qblocks[i]["items"])
                    i += 1
                    if len(grp) > 0 and i < nq and qblocks[i].get("flush_before"):
                        break
                assert grp, "qblock too large"
                sti = 1 - nxt("st")
                sbank, skey = (pY[0][:, sti * 512:(sti + 1) * 512], Yk[0][sti])
                pti = nxt("pt")

                def st_mm(e, grp=grp, sbank=sbank):
                    ins = None
                    c = 0
                    first = True
                    for qb in grp:
                        for (k_ap, v_ap, b_ap) in qb["items"]:
                            if b_ap is not None:
                                e.matmul(sbank[:, c:c + qb["n"]], ident[:], b_ap, start=first, stop=False,
                                         skip_group_check=True)
                                first = False
                            c += qb["n"]
                    c = 0
                    for qb in grp:
                        for (k_ap, v_ap, b_ap) in qb["items"]:
                            ins = e.matmul(sbank[:, c:c + qb["n"]], k_ap, qb["q"],
                                           start=(first and b_ap is None), stop=True, skip_group_check=True)
                            if b_ap is None:
                                first = False
                            c += qb["n"]
                    return ins
                op("pe", st_mm, reads=list(q_keys) + list(k_keys) + list(bias_keys) + ["ident"], writes=[skey])
                op("act", lambda e, cols=cols, sbank=sbank, pti=pti: e.activation(PT[pti][:, 0:cols], sbank[:, 0:cols], AF.Exp),
                   reads=[skey], writes=[("PT", pti)])
                def stage2(grp=grp, pti=pti):
                    pv_pending = []
                    c = 0
                    for qb in grp:
                        offs = []
                        for _ in qb["items"]:
                            offs.append(c)
                            c += qb["n"]
                        pv_pending.append((qb, pti, offs))
                    j = 0
                    while j < len(pv_pending):
                        tot = 0
                        piece = []
                        while j < len(pv_pending) and tot + pv_pending[j][0]["n"] <= 512:
                            piece.append(pv_pending[j])
                            tot += pv_pending[j][0]["n"]
                            j += 1
                        pvi = nxt("pv")
                        pbank, pkey = (pY[1][:, pvi * 512:(pvi + 1) * 512], Yk[1][pvi])

                        def pv_mm(e, piece=piece, pbank=pbank):
                            ins = None
                            c2 = 0
                            for (qb, pti2, offs) in piece:
                                for ii, (k_ap, v_ap, b_ap) in enumerate(qb["items"]):
                                    ins = e.matmul(pbank[:, c2:c2 + qb["n"]], v_ap,
                                                   PT[pti2][:, offs[ii]:offs[ii] + qb["n"]],
                                                   start=(ii == 0), stop=(ii == len(qb["items"]) - 1),
                                                   skip_group_check=True)
                                c2 += qb["n"]
                            return ins
                        op("pe", pv_mm, reads=[("PT", piece[0][1])] + list(va_keys), writes=[pkey])
                        c2 = 0
                        for (qb, pti2, offs) in piece:
                            n = qb["n"]
                            if qb["add"]:
                                op("dve", lambda e, qb=qb, c2=c2, n=n, pbank=pbank: e.tensor_tensor(
                                    qb["dst"], pbank[:, c2:c2 + n], qb["dst"], ALU.add),
                                   reads=[pkey, acc_key], writes=[acc_key])
                            else:
                                op("dve", lambda e, qb=qb, c2=c2, n=n, pbank=pbank: e.tensor_copy(
                                    qb["dst"], pbank[:, c2:c2 + n]),
                                   reads=[pkey], writes=[acc_key])
                            c2 += n
                    pv_pending = []

                if prev_stage2 is not None:
                    prev_stage2()
                prev_stage2 = stage2

            if prev_stage2 is not None:
                prev_stage2()

        def head_epilogue(hh, chunk_slot, acc_key, ntok=S):
            u = slice(0, 64) if hh == 0 else slice(64, 128)
            dn = slice(64, 128) if hh == 0 else slice(0, 64)
            for b in range(ntok // 512):
                cs = slice(b * 512, (b + 1) * 512)
                ri = nxt("rs")
                op("pool", lambda e, ri=ri, cs=cs: e.tensor_copy(rs[ri][u, :], acc[dn, cs]),
                   reads=[acc_key], writes=[("rsl", ri), ("rs", ri)])
                op("dve", lambda e, ri=ri: e.reciprocal(rs[ri][u, :], rs[ri][u, :]),
                   reads=[("rsl", ri)], writes=[("rs", ri)])
                op("dve", lambda e, ri=ri, cs=cs: e.tensor_tensor(oT[u, chunk_slot, cs], acc[u, cs], rs[ri][u, :], ALU.mult),
                   reads=[acc_key, ("rs", ri)], writes=[("oT", chunk_slot, hh)])

        def group_finalize(l, chunks, width):
            ng = len(chunks)
            dma("pool", "Wo", [(Wo[:, 0:ng, :], w_out_d[l, :, chunks[0]:chunks[0] + ng, :])], writes=["Wo"])
            for b in range(4):
                sbank, skey = B[2]
                for ci in range(ng):
                    si = nxt("sq")
                    op("act", lambda e, ci=ci, si=si, b=b: e.activation(sq[si][:], oT[:, ci, b * 512:(b + 1) * 512], AF.Square),
                       reads=[("oT", ci, 0), ("oT", ci, 1)], writes=[("sq", si)])
                    op("pe", lambda e, ci=ci, si=si: e.matmul(sbank[:], ones[:], sq[si][:], start=(ci == 0), stop=(ci == ng - 1)),
                       reads=[("sq", si), "ones"], writes=[skey])
                ri = nxt("rs")
                op("act", lambda e, ri=ri: e.activation(rs[ri][:], sbank[:], AF.Ln, bias=float(EPS), scale=1.0 / width),
                   reads=[skey], writes=[("rsl", ri), ("rs", ri)])
                op("act", lambda e, ri=ri: e.activation(rs[ri][:], rs[ri][:], AF.Exp, scale=-0.5),
                   reads=[("rsl", ri)], writes=[("rs", ri)])
                for ci in range(ng):
                    gcol = og[:, l * 8 + chunks[ci]:l * 8 + chunks[ci] + 1]
                    op("dve", lambda e, ci=ci, gcol=gcol, ri=ri, b=b: e.scalar_tensor_tensor(
                        oT[:, ci, b * 512:(b + 1) * 512], oT[:, ci, b * 512:(b + 1) * 512], gcol, rs[ri][:],
                        ALU.mult, ALU.mult),
                       reads=[("oT", ci, 0), ("oT", ci, 1), ("rs", ri), "og"], writes=[("mix", ci, b)])
            for t in range(NT):
                yi = nxt("y")

                def mm(e, t=t, yi=yi):
                    ins = None
                    for half in range(2):
                        for ci in range(ng):
                            ins = e.matmul(pY[yi][:, half * 512:(half + 1) * 512], oT[:, ci, t * 128:(t + 1) * 128],
                                           Wo[:, ci, half * 512:(half + 1) * 512], start=(ci == 0), stop=(ci == ng - 1))
                    return ins
                op("pe", mm, reads=[("mix", ci, t // 4) for ci in range(ng)] + ["Wo"], writes=list(Yk[yi]))
                op("dve", lambda e, t=t, yi=yi: e.tensor_tensor(x_sb[:, t, :], pY[yi][:], x_sb[:, t, :], ALU.add),
                   reads=list(Yk[yi]) + [("x", t)], writes=[("x", t)])
            for ci in range(ng):
                for hh in range(2):
                    sc.res[("oT", ci, hh)] = sc.res[("mix", ci, 3)]

        def load_pair_w(l, chunk_ids):
            wi = nxt("wt", len(Wt))
            dma("pool", "Wt%d" % wi, [(Wt[wi][:, j, :, :], w_in_d[l, cid]) for j, cid in enumerate(chunk_ids)],
                writes=[("Wt", wi)])
            return wi

        def attn_phase(s, l):
            for i in range(2):
                op("pool", lambda e, i=i: e.memset(VA[i], 1.0), writes=[("VA", i), ("VA", i, "b")])
            op("pool", lambda e: e.memset(VAM, 1.0), writes=["VAM", ("VAM", "b")])
            if stop == "memset":
                return
            mem_path(s, l)
            if stop == "mem" or (stop and stop[0] in "nm" and stop != "norm"):
                return
            norm_to_hT(x_sb, "x", NT, g_mix_d[l:l + 1, :], hT, "hT")
            if stop == "norm":
                return
            hkeys = [("hT", t) for t in range(NT)]
            for grp_name in ("A", "B", "M"):
                if grp_name == "M":
                    npair = 2
                else:
                    npair = 3
                for pr in range(npair):
                    if grp_name == "A":
                        cids = [pr, 3 + pr, 6 + pr]
                        gi = (0, 1)
                    elif grp_name == "B":
                        cids = [9 + pr, 12 + pr, 15 + pr]
                        gi = (2, 3)
                    else:
                        cids = [18 + pr]
                        gi = (4, 5)
                    wi = load_pair_w(l, cids)
                    proj_chunk(lambda kc, wi=wi: Wt[wi][:, 0, kc, :], ("Wt", wi), hT, "hT", S,
                               qk_consume(qT, "qT", qkg8[:, l * 6 + gi[0]:l * 6 + gi[0] + 1]))
                    qkeys = [("qT", b) for b in range(4)]
                    if grp_name != "M":
                        proj_chunk(lambda kc, wi=wi: Wt[wi][:, 1, kc, :], ("Wt", wi), hT, "hT", S,
                                   qk_consume(kT, "kT", qkg[:, l * 6 + gi[1]:l * 6 + gi[1] + 1]))
                        proj_chunk(lambda kc, wi=wi: Wt[wi][:, 2, kc, :], ("Wt", wi), hT, "hT", S,
                                   copy_consume(vT, "vT"))
                        kkeys = [("kT", b) for b in range(4)]
                        vkeys = [("vT", b) for b in range(4)]
                        hb = 0 if grp_name == "A" else 1
                        if grp_name == "A":
                            dma("pool", "biasA", [(biasA[:, hh, :, :], bias_a_d[2 * pr + hh].rearrange("p k q -> k p q"))
                                                  for hh in range(2)], writes=["biasA"])
                    if grp_name == "A":
                        for hh in range(2):
                            pass
                        accs = {}
                        for hh in range(2):
                            hs = slice(64 * hh, 64 * hh + 64)
                            for p, dil in enumerate((1, 4, 16)):
                                vi = p % 2 if hh == 0 else None
                        for hh in range(2):
                            hs = slice(64 * hh, 64 * hh + 64)
                            for p, dil in enumerate((1, 4, 16)):
                                L = S // dil
                                nkt = L // 128
                                vi = nxt("va")
                                va = VA[vi]
                                toks = [ssl(dil * 128 * j + r, 128, dil)
                                        for r in range(dil) for j in range(nkt)]
                                if hh == 0 or True:
                                    build_va(va, ("VA", vi), toks, vT, vkeys)
                                qbs = []
                                for r in range(dil):
                                    for i in range(nkt + 1):
                                        lo_l = 128 * i - 64
                                        q0 = max(lo_l, 0)
                                        q1 = min(lo_l + 128, L)
                                        n = q1 - q0
                                        qo = q0 - lo_l
                                        items = []
                                        if i >= 1:
                                            j = i - 1
                                            ksl = ssl(dil * 128 * j + r, 128, dil)
                                            items.append((kT[hs, ksl], va[:, r * nkt + j, hh, :],
                                                          biasA[:, hh, p, qo:qo + n]))
                                        if i <= nkt - 1:
                                            j = i
                                            ksl = ssl(dil * 128 * j + r, 128, dil)
                                            items.append((kT[hs, ksl], va[:, r * nkt + j, hh, :],
                                                          biasA[:, hh, p, 128 + qo:128 + qo + n]))
                                        qsl = ssl(dil * q0 + r, n, dil)
                                        qbs.append(dict(q=qT[hs, qsl], n=n, items=items, dst=acc[:, qsl],
                                                        add=(p > 0)))
                                attention(qbs, qkeys, kkeys, [("VA", vi), ("VA", vi, "b")], ["biasA"], "acc")
                            head_epilogue(hh, pr, "acc")
                    elif grp_name == "B":
                        vi = nxt("va")
                        va = VA[vi]
                        toks = [slice(128 * j, 128 * j + 128) for j in range(NT)]
                        build_va(va, ("VA", vi), toks, vT, vkeys)
                        for hh in range(2):
                            hs = slice(64 * hh, 64 * hh + 64)
                            dma("pool", "biasB", [(biasB, bias_b_d[l, 2 * pr + hh])], writes=["biasB"])
                            qbs = []
                            for T in range(NT):
                                items = []
                                for J in na_keytiles(T):
                                    v = na_variant(T, J)
                                    items.append((kT[hs, 128 * J:128 * J + 128], va[:, J, hh, :], biasB[:, v, :]))
                                qsl = slice(128 * T, 128 * T + 128)
                                qbs.append(dict(q=qT[hs, qsl], n=128, items=items[:4], dst=acc[:, qsl], add=False))
                                if len(items) > 4:
                                    qbs.append(dict(q=qT[hs, qsl], n=128, items=items[4:], dst=acc[:, qsl], add=True))
                            attention(qbs, qkeys, kkeys, [("VA", vi), ("VA", vi, "b")], ["biasB"], "acc")
                            head_epilogue(hh, pr, "acc")
                    else:
                        for hh in range(2):
                            hs = slice(64 * hh, 64 * hh + 64)
                            qbs = []
                            for b in range(4):
                                qsl = slice(512 * b, 512 * b + 512)
                                items = [(kmT[hs, pr, 128 * j:128 * j + 128], VAM[:, pr, j, hh, :], None) for j in range(2)]
                                qbs.append(dict(q=qT[hs, qsl], n=512, items=items[:1], dst=acc[:, qsl], add=False))
                                qbs.append(dict(q=qT[hs, qsl], n=512, items=items[1:], dst=acc[:, qsl], add=True))
                            attention(qbs, qkeys, [("kmT", pr)], ["VAM", ("VAM", "b")], [], "acc")
                            head_epilogue(hh, pr, "acc")
                if stop == grp_name + "attn":
                    return
                if grp_name == "A":
                    group_finalize(l, [0, 1, 2], 384)
                elif grp_name == "B":
                    group_finalize(l, [3, 4, 5], 384)
                else:
                    group_finalize(l, [6, 7], 256)
                if stop == grp_name:
                    return

        def mem_path(s, l):
            dma("sp", "mem", [(mem_sb, mem_d[s].rearrange("(t p) d -> p t d", p=128))],
                reads=[], writes=[("mem", 0), ("mem", 1), "acc"])
            norm_to_hT(mem_sb, "mem", 2, g_mem_d[l:l + 1, :], hT, "hT")
            if stop and stop.startswith("n"):
                return
            rd = {}
            for t in range(2):
                for k, v in sc.res[("mem", t)]["r"].items():
                    if k not in rd or rd[k][2] < v[2]:
                        rd[k] = v
            sc.res["acc"] = dict(w=None, r=rd)
            for j in range(4):
                if stop == "m0" or (stop == "m1" and j >= 1) or (stop == "m2" and j >= 2) or (stop == "m3" and j >= 3):
                    return
                wi = nxt("wt", len(Wt))
                dma("pool", "Wt%d" % wi, [(Wt[wi][:, 0, :, :], w_mem_d[l, j])], writes=[("Wt", wi)])
                if j < 2:
                    def consume(bank, bkey, b, n, j=j):
                        qk_consume(kmT[:, j, :], ("kmTb", j), qkg[:, l * 6 + 5:l * 6 + 6])(bank, bkey, b, n)
                        sc.res[("kmT", j)] = sc.res[(("kmTb", j), 0)]
                    proj_chunk(lambda kc, wi=wi: Wt[wi][:, 0, kc, :], ("Wt", wi), hT, "hT", MEM, consume)
                else:
                    proj_chunk(lambda kc, wi=wi: Wt[wi][:, 0, kc, :], ("Wt", wi), hT, "hT", MEM,
                               copy_consume(vmT, "vmT"))
                    pr = j - 2
                    toks = [slice(0, 128), slice(128, 256)]

                    def tr(e):
                        ins = None
                        for t in range(2):
                            ins = e.transpose(pTr[:, t, :], vmT[:, toks[t]], ident[:])
                        return ins
                    op("pe", tr, reads=[("vmT", 0), "ident"], writes=[BK_TR])
                    op("dve", lambda e, pr=pr: e.tensor_copy(VAM[:, pr, :, 0, 0:64], pTr[:, 0:2, 0:64]),
                       reads=[BK_TR], writes=["VAM"])
                    op("dve", lambda e, pr=pr: e.tensor_copy(VAM[:, pr, :, 1, 64:128], pTr[:, 0:2, 64:128]),
                       reads=[BK_TR], writes=[("VAM", "b")])

        def moe_phase(s, l):
            norm_to_hT(x_sb, "x", NT, g_ffn_d[l:l + 1, :], hT, "hT")
            dma("pool", "Wr", [(Wr[:], w_rt_d[l])], writes=["Wr"])
            dma("sp", "brt", [(brt[:], b_rt_d[l:l + 1, :].partition_broadcast(128))], writes=["brt"])
            for t in range(NT):
                bank, bkey = B[2]

                def mm(e, t=t):
                    ins = None
                    for kc in range(8):
                        ins = e.matmul(bank[:, 0:36], hT[:, kc, t * 128:(t + 1) * 128], Wr[:, kc, :],
                                       start=(kc == 0), stop=(kc == 7))
                    return ins
                op("pe", mm, reads=[("hT", t), "Wr"], writes=[bkey])
                op("dve", lambda e: e.tensor_tensor(lg[:], bank[:, 0:36], brt[:], ALU.add),
                   reads=[bkey, "brt"], writes=["lg"])
                op("dve", lambda e: e.reduce_max(rt_s[:, 0:1], lg[:, 0:4], AX.X), reads=["lg"], writes=["rt0"])
                op("dve", lambda e: e.tensor_scalar_mul(rt_s[:, 1:2], rt_s[:, 0:1], -1.0), reads=["rt0"], writes=["rt1"])
                op("act", lambda e: e.activation(junk[:, 0:4], lg[:, 0:4], AF.Exp, bias=rt_s[:, 1:2],
                                                 accum_out=rt_s[:, 2:3]),
                   reads=["lg", "rt1"], writes=["junk", "rt2"])
                op("dve", lambda e: e.reciprocal(rt_s[:, 3:4], rt_s[:, 2:3]), reads=["rt2"], writes=["rt3"])
                op("dve", lambda e: e.tensor_scalar(rt_s[:, 4:8], lg[:, 0:4], rt_s[:, 0:1], NEG * 10, ALU.is_lt, ALU.mult),
                   reads=["lg", "rt0"], writes=["rt4"])
                for g in range(4):
                    op("dve", lambda e, g=g: e.tensor_scalar_add(lmask[:, g * 8:(g + 1) * 8], lg[:, 4 + g * 8:12 + g * 8],
                                                                 rt_s[:, 4 + g:5 + g]),
                       reads=["lg", "rt4"], writes=[("lmask", g)])
                lm = [("lmask", g) for g in range(4)]
                op("dve", lambda e: e.reduce_max(mx8[:, 0:1], lmask[:], AX.X), reads=lm, writes=["mx8a"])
                op("dve", lambda e: e.tensor_scalar(eq2[:], lmask[:], mx8[:, 0:1], NEG * 10, ALU.is_equal, ALU.mult),
                   reads=lm + ["mx8a"], writes=["eq2"])
                op("dve", lambda e: e.tensor_tensor(eq1[:], lmask[:], eq2[:], ALU.add), reads=lm + ["eq2"], writes=["eq1"])
                op("dve", lambda e: e.reduce_max(mx8[:, 1:2], eq1[:], AX.X), reads=["eq1"], writes=["mx8"])
                op("dve", lambda e: e.tensor_tensor(rt_s[:, 8:9], mx8[:, 1:2], mx8[:, 0:1], ALU.subtract),
                   reads=["mx8", "mx8a"], writes=["rt8"])
                op("act", lambda e: e.activation(rt_s[:, 9:10], rt_s[:, 8:9], AF.Exp), reads=["rt8"], writes=["rt9"])
                op("dve", lambda e: e.tensor_scalar_add(rt_s[:, 9:10], rt_s[:, 9:10], 1.0), reads=["rt9"], writes=["rt9"])
                op("dve", lambda e: e.reciprocal(rt_s[:, 12:13], rt_s[:, 9:10]), reads=["rt9"], writes=["rt12"])
                op("dve", lambda e: e.tensor_tensor(rt_s[:, 10:11], rt_s[:, 3:4], rt_s[:, 12:13], ALU.mult),
                   reads=["rt12", "rt3"], writes=["rt10"])
                op("dve", lambda e: e.tensor_tensor(rt_s[:, 11:12], rt_s[:, 3:4], rt_s[:, 10:11], ALU.subtract),
                   reads=["rt10", "rt3"], writes=["rt11"])
                op("dve", lambda e: e.tensor_scalar(eq1[:], lmask[:], mx8[:, 0:1], rt_s[:, 10:11], ALU.is_equal, ALU.mult),
                   reads=lm + ["mx8", "mx8a", "rt10"], writes=["eq1"])
                op("dve", lambda e: e.tensor_scalar(eq2[:], lmask[:], mx8[:, 1:2], rt_s[:, 11:12], ALU.is_equal, ALU.mult),
                   reads=lm + ["mx8", "rt11"], writes=["eq2"])
                op("dve", lambda e, t=t: e.tensor_tensor(gates[:, t, :], eq1[:], eq2[:], ALU.add),
                   reads=["eq1", "eq2"], writes=[("gates", t)])
            def load_e(e_):
                wi = nxt("wm")
                dma("pool", "We%d" % wi, [(Wg[wi][:], w_gate_d[l, e_]), (Wu[wi][:], w_up_d[l, e_]),
                                          (Wd[wi][:], w_down_d[l, e_])], writes=[("We", wi)])
                return wi
            wi_next = load_e(0)
            for e_ in range(n_experts):
                wi = wi_next
                if e_ + 1 < n_experts:
                    wi_next = load_e(e_ + 1)
                for b in range(4):
                    hi_ = nxt("hid")
                    for dc in range(4):
                        gb = nxt("proj")
                        gbank, gkey = B[gb]
                        ubank, ukey = B[2] if False else (None, None)
                        ui = nxt("st")
                        ubank, ukey = (pY[0][:, ui * 512:(ui + 1) * 512], Yk[0][ui])

                        def mmg(e, wi=wi, b=b, dc=dc, gbank=gbank):
                            ins = None
                            for kc in range(8):
                                ins = e.matmul(gbank[:], Wg[wi][:, kc, dc * 128:(dc + 1) * 128], hT[:, kc, b * 512:(b + 1) * 512],
                                               start=(kc == 0), stop=(kc == 7))
                            return ins

                        def mmu(e, wi=wi, b=b, dc=dc, ubank=ubank):
                            ins = None
                            for kc in range(8):
                                ins = e.matmul(ubank, Wu[wi][:, kc, dc * 128:(dc + 1) * 128], hT[:, kc, b * 512:(b + 1) * 512],
                                               start=(kc == 0), stop=(kc == 7))
                            return ins
                        hk = [("hT", t) for t in range(4 * b, 4 * b + 4)]
                        op("pe", mmg, reads=[("We", wi)] + hk, writes=[gkey])
                        op("pe", mmu, reads=[("We", wi)] + hk, writes=[ukey])
                        si = nxt("sg")
                        op("act", lambda e, si=si, gbank=gbank: e.activation(sg[si][:], gbank[:], AF.Silu),
                           reads=[gkey], writes=[("sg", si)])
                        op("dve", lambda e, si=si, ubank=ubank, hi_=hi_, dc=dc: e.tensor_tensor(
                            hid[hi_][:, dc, :], ubank, sg[si][:], ALU.mult),
                           reads=[ukey, ("sg", si)], writes=[("hid", hi_, dc)])
                    for tt in range(4):
                        t = 4 * b + tt
                        yi = 1

                        def mmd(e, wi=wi, hi_=hi_, tt=tt):
                            ins = None
                            for half in range(2):
                                for dc in range(4):
                                    ins = e.matmul(pY[1][:, half * 512:(half + 1) * 512], hid[hi_][:, dc, tt * 128:(tt + 1) * 128],
                                                   Wd[wi][:, dc, half * 512:(half + 1) * 512], start=(dc == 0), stop=(dc == 3))
                            return ins
                        op("pe", mmd, reads=[("We", wi)] + [("hid", hi_, dc) for dc in range(4)], writes=list(Yk[1]))
                        op("dve", lambda e, t=t, e_=e_: e.scalar_tensor_tensor(
                            x_sb[:, t, :], pY[1][:], gates[:, t, e_:e_ + 1], x_sb[:, t, :], ALU.mult, ALU.add),
                           reads=list(Yk[1]) + [("x", t), ("gates", t)], writes=[("x", t)])

        def moe_phase_sparse(s, l):
            dma("pool", "Wr", [(Wr[:], w_rt_d[l])], writes=["Wr"])
            dma("sp", "brt", [(brt[:], b_rt_d[l:l + 1, :].partition_broadcast(128))], writes=["brt"])
            dma("sp", "gbc", [(gbc[:], g_ffn_d[l:l + 1, :].partition_broadcast(128))], writes=["gbc"])

            def load_e(e_):
                wi = nxt("wm")
                dma("pool", "We%d" % wi, [(Wg[wi][:], w_gate_d[l, e_]), (Wu[wi][:], w_up_d[l, e_]),
                                          (Wd[wi][:], w_down_d[l, e_])], writes=[("We", wi)])
                return wi
            wi_next = load_e(0)
            scat_keys = []
            for t in range(NT):
                op("act", lambda e, t=t: e.activation(junk[:], x_sb[:, t, :], AF.Square, accum_out=ssum[:, t:t + 1]),
                   reads=[("x", t)], writes=["junk", ("ssum", t)])
                op("act", lambda e, t=t: e.activation(rstd[:, t:t + 1], ssum[:, t:t + 1], AF.Ln, bias=float(EPS), scale=1.0 / D),
                   reads=[("ssum", t)], writes=[("rstdq", t), ("rstd", t)])
                op("act", lambda e, t=t: e.activation(rstd[:, t:t + 1], rstd[:, t:t + 1], AF.Exp, scale=-0.5),
                   reads=[("rstdq", t)], writes=[("rstd", t)])
                hb = nxt("ht")
                op("dve", lambda e, t=t, hb=hb: e.scalar_tensor_tensor(htok[hb][:], x_sb[:, t, :], rstd[:, t:t + 1], gbc[:], ALU.mult, ALU.mult),
                   reads=[("x", t), ("rstd", t), "gbc"], writes=[("htok", hb)])

                def tr(e, hb=hb):
                    ins = None
                    for c in range(8):
                        ins = e.transpose(pTr[:, c, :], htok[hb][:, c * 128:(c + 1) * 128], ident[:])
                    return ins
                op("pe", tr, reads=[("htok", hb), "ident"], writes=[BK_TR])
                op("act", lambda e, t=t: e.copy(hT[:, :, t * 128:(t + 1) * 128], pTr[:]), reads=[BK_TR], writes=[("hT", t)])
                bank, bkey = B[2]

                def mm(e, t=t):
                    ins = None
                    for kc in range(8):
                        ins = e.matmul(bank[:, 0:36], hT[:, kc, t * 128:(t + 1) * 128], Wr[:, kc, :], start=(kc == 0), stop=(kc == 7))
                    return ins
                op("pe", mm, reads=[("hT", t), "Wr"], writes=[bkey])
                op("dve", lambda e: e.tensor_tensor(lg[:], bank[:, 0:36], brt[:], ALU.add), reads=[bkey, "brt"], writes=["lg"])
                op("dve", lambda e: e.reduce_max(rt_s[:, 0:1], lg[:, 0:4], AX.X), reads=["lg"], writes=["rt0"])
                op("dve", lambda e: e.tensor_scalar_mul(rt_s[:, 1:2], rt_s[:, 0:1], -1.0), reads=["rt0"], writes=["rt1"])
                op("act", lambda e: e.activation(junk[:, 0:4], lg[:, 0:4], AF.Exp, bias=rt_s[:, 1:2], accum_out=rt_s[:, 2:3]),
                   reads=["lg", "rt1"], writes=["junk", "rt2"])
                op("dve", lambda e: e.reciprocal(rt_s[:, 3:4], rt_s[:, 2:3]), reads=["rt2"], writes=["rt3"])
                op("dve", lambda e: e.tensor_scalar(rt_s[:, 4:8], lg[:, 0:4], rt_s[:, 0:1], NEG * 10, ALU.is_lt, ALU.mult),
                   reads=["lg", "rt0"], writes=["rt4"])
                for g in range(4):
                    op("dve", lambda e, g=g: e.tensor_scalar_add(lmask[:, g * 8:(g + 1) * 8], lg[:, 4 + g * 8:12 + g * 8], rt_s[:, 4 + g:5 + g]),
                       reads=["lg", "rt4"], writes=[("lmask", g)])
                lm = [("lmask", g) for g in range(4)]
                op("dve", lambda e: e.reduce_max(mx8[:, 0:1], lmask[:], AX.X), reads=lm, writes=["mx8a"])
                op("dve", lambda e: e.tensor_scalar(eq1[:], lmask[:], mx8[:, 0:1], None, ALU.is_equal), reads=lm + ["mx8a"], writes=["eq1"])
                op("dve", lambda e: e.scalar_tensor_tensor(lmask[:], eq1[:], NEG * 10, lmask[:], ALU.mult, ALU.add),
                   reads=lm + ["eq1"], writes=lm)
                op("dve", lambda e: e.reduce_max(mx8[:, 1:2], lmask[:], AX.X), reads=lm, writes=["mx8"])
                op("dve", lambda e: e.tensor_scalar(eq2[:], lmask[:], mx8[:, 1:2], None, ALU.is_equal), reads=lm + ["mx8"], writes=["eq2"])
                op("dve", lambda e: e.tensor_tensor(rt_s[:, 8:9], mx8[:, 1:2], mx8[:, 0:1], ALU.subtract), reads=["mx8", "mx8a"], writes=["rt8"])
                op("act", lambda e: e.activation(rt_s[:, 9:10], rt_s[:, 8:9], AF.Exp), reads=["rt8"], writes=["rt9"])
                op("dve", lambda e: e.tensor_scalar_add(rt_s[:, 9:10], rt_s[:, 9:10], 1.0), reads=["rt9"], writes=["rt9"])
                op("dve", lambda e: e.reciprocal(rt_s[:, 12:13], rt_s[:, 9:10]), reads=["rt9"], writes=["rt12"])
                op("dve", lambda e, t=t: e.tensor_tensor(g01[:, t, 0:1], rt_s[:, 3:4], rt_s[:, 12:13], ALU.mult),
                   reads=["rt12", "rt3"], writes=[("g0", t)])
                op("dve", lambda e, t=t: e.tensor_tensor(g01[:, t, 1:2], rt_s[:, 3:4], g01[:, t, 0:1], ALU.subtract),
                   reads=[("g0", t), "rt3"], writes=[("g1", t)])
                op("dve", lambda e, t=t: e.tensor_tensor(Mall[:, t, :], eq1[:], eq2[:], ALU.add), reads=["eq1", "eq2"], writes=[("M", t)])
                def rk(e, t=t):
                    ins = None
                    for tp in range(t):
                        ins = e.matmul(bank[:, 64:64 + NE], ones[:], Mall[:, tp, :], start=(tp == 0), stop=False)
                    ins = e.matmul(bank[:, 64:64 + NE], triu[:], Mall[:, t, :], start=(t == 0), stop=True)
                    return ins
                op("pe", rk, reads=[("M", tp) for tp in range(t + 1)] + ["ones", "triu", "lg"], writes=[bkey])
                op("dve", lambda e: e.tensor_tensor(slotf[:], bank[:, 64:64 + NE], eoff[:], ALU.add), reads=[bkey, "eoff"], writes=["slotf"])
                op("dve", lambda e: e.tensor_tensor(eq1[:], eq1[:], slotf[:], ALU.mult), reads=["eq1", "slotf", ("M", t)], writes=["eq1"])
                op("dve", lambda e: e.tensor_tensor(eq2[:], eq2[:], slotf[:], ALU.mult), reads=["eq2", "slotf", ("M", t)], writes=["eq2"])
                op("dve", lambda e: e.reduce_sum(slot01f[:, 0:1], eq1[:], AX.X), reads=["eq1"], writes=["s0f"])
                op("dve", lambda e: e.reduce_sum(slot01f[:, 1:2], eq2[:], AX.X), reads=["eq2"], writes=["s1f"])
                op("dve", lambda e, t=t: e.tensor_copy(slot01[:, t, :], slot01f[:]), reads=["s0f", "s1f"], writes=[("slot", t)])
                for k in range(2):
                    def sc_fn(e, sem, t=t, k=k, hb=hb):
                        e.indirect_dma_start(out=Hs_d[:, :], out_offset=bass.IndirectOffsetOnAxis(ap=slot01[:, t, k:k + 1], axis=0),
                                             in_=htok[hb][:, :], in_offset=None).then_inc(sem, 16)
                    op("pool", sc_fn, reads=[("slot", t), ("htok", hb)], writes=[("Hs", t, k)], dma_key="hs", ndma=1)
                    scat_keys.append(("Hs", t, k))
            bank, bkey = B[2]

            def cntmm(e):
                ins = None
                for tp in range(NT):
                    ins = e.matmul(bank[:, 128:128 + NE], ones[:], Mall[:, tp, :], start=(tp == 0), stop=(tp == NT - 1))
                return ins
            op("pe", cntmm, reads=[("M", tp) for tp in range(NT)] + ["ones", "slotf"], writes=[bkey])
            op("dve", lambda e: e.tensor_scalar(cntf[:, 0:NE], bank[:, 128:128 + NE], 256.5, None, ALU.is_gt), reads=[bkey], writes=["cntf"])
            op("dve", lambda e: e.tensor_scalar(cntf[:, NE + 2:2 * NE], bank[:, 128:128 + NE - 2], 0.0, None, ALU.mult), reads=[bkey], writes=["cntf3"])
            op("dve", lambda e: e.reduce_max(rt_s[:, 13:14], bank[:, 128:128 + NE], AX.X), reads=[bkey], writes=["rt13"])
            op("dve", lambda e: e.tensor_scalar(cntf[:, NE:NE + 1], rt_s[:, 13:14], 512.5, None, ALU.is_gt), reads=["rt13"], writes=["cntf2"])
            op("dve", lambda e: e.tensor_scalar(cntf[:, NE + 1:NE + 2], rt_s[:, 13:14], 1024.5, None, ALU.is_gt), reads=["rt13"], writes=["cntf4"])
            op("dve", lambda e: e.tensor_copy(flag_i[:], cntf[:]), reads=["cntf", "cntf2", "cntf3", "cntf4"], writes=["flag"])

            ystore_keys = []

            def prefetch_hs(e_, j0):
                r0 = e_ * S + j0 * 128
                hi = e_ % 2
                dma("sp", "hsl%d" % hi, [(hs_tok[hi], Hs_d[r0:r0 + 256, :].rearrange("(j p) d -> p j d", p=128))],
                    reads=scat_keys, writes=[("hst", hi)])
                return hi

            def expert_tiles(e_, wi, j0, hi=None):
                r0 = e_ * S + j0 * 128
                if hi is None:
                    hi = prefetch_hs(e_, j0)
                he = nxt("hte")
                for j in range(2):
                    def tr(e, j=j, hi=hi):
                        ins = None
                        for c in range(8):
                            ins = e.transpose(pTr[:, c, :], hs_tok[hi][:, j, c * 128:(c + 1) * 128], ident[:])
                        return ins
                    op("pe", tr, reads=[("hst", hi), "ident"], writes=[BK_TR])
                    op("act", lambda e, j=j, he=he: e.copy(hTe[he][:, :, j * 128:(j + 1) * 128], pTr[:]),
                       reads=[BK_TR], writes=[("hte", he, j)])
                hk = [("hte", he, 0), ("hte", he, 1)]
                hi_ = nxt("hid")
                for dc in range(4):
                    gb = nxt("proj")
                    gbank, gkey = B[gb]
                    ui = nxt("st")
                    ubank, ukey = (pY[0][:, ui * 512:ui * 512 + 256], Yk[0][ui])

                    def mmg(e, dc=dc, gbank=gbank):
                        ins = None
                        for kc in range(8):
                            ins = e.matmul(gbank[:, 0:256], Wg[wi][:, kc, dc * 128:(dc + 1) * 128], hTe[he][:, kc, :],
                                           start=(kc == 0), stop=(kc == 7))
                        return ins

                    def mmu(e, dc=dc, ubank=ubank):
                        ins = None
                        for kc in range(8):
                            ins = e.matmul(ubank, Wu[wi][:, kc, dc * 128:(dc + 1) * 128], hTe[he][:, kc, :],
                                           start=(kc == 0), stop=(kc == 7))
                        return ins
                    op("pe", mmg, reads=[("We", wi)] + hk, writes=[gkey])
                    op("pe", mmu, reads=[("We", wi)] + hk, writes=[ukey])
                    si = nxt("sg")
                    op("act", lambda e, si=si, gbank=gbank: e.activation(sg[si][:, 0:256], gbank[:, 0:256], AF.Silu),
                       reads=[gkey], writes=[("sg", si)])
                    op("dve", lambda e, si=si, ubank=ubank, dc=dc: e.tensor_tensor(hid[hi_][:, dc, 0:256], ubank, sg[si][:, 0:256], ALU.mult),
                       reads=[ukey, ("sg", si)], writes=[("hid", hi_, dc)])
                for j in range(2):
                    def mmd(e, j=j):
                        ins = None
                        for half in range(2):
                            for dc in range(4):
                                ins = e.matmul(pY[1][:, half * 512:(half + 1) * 512], hid[hi_][:, dc, j * 128:(j + 1) * 128],
                                               Wd[wi][:, dc, half * 512:(half + 1) * 512], start=(dc == 0), stop=(dc == 3))
                        return ins
                    op("pe", mmd, reads=[("We", wi)] + [("hid", hi_, dc) for dc in range(4)], writes=list(Yk[1]))
                    yo = nxt("yo")
                    if j == 0:
                        op("act", lambda e, yo=yo: e.copy(yout[yo], pY[1][:]), reads=list(Yk[1]), writes=[("yo", yo)])
                    else:
                        op("dve", lambda e, yo=yo: e.tensor_copy(yout[yo], pY[1][:]), reads=list(Yk[1]), writes=[("yo", yo)])
                    rr0 = r0 + j * 128
                    dma("sp", "ys", [(Ys_d[rr0:rr0 + 128, :], yout[yo])], reads=[("yo", yo)], writes=[("Ys", e_, j0 + j)])
                    ystore_keys.append(("Ys", e_, j0 + j))

            hi_next = prefetch_hs(0, 0)
            for e_ in range(n_experts):
                wi = wi_next
                hi_cur = hi_next
                if e_ + 1 < n_experts:
                    wi_next = load_e(e_ + 1)
                    hi_next = prefetch_hs(e_ + 1, 0)
                expert_tiles(e_, wi, 0, hi_cur)
                if guard:
                    sc.begin_guard(flag_i[0:1, e_:e_ + 1], "flag")
                    expert_tiles(e_, wi, 2)
                    sc.end_guard()
            if guard:
                for fcol, jlo, jhi in ((NE, 4, 8), (NE + 1, 8, NT)):
                    sc.begin_guard(flag_i[0:1, fcol:fcol + 1], "flag")
                    wi_next = load_e(0)
                    for e_ in range(n_experts):
                        wi = wi_next
                        if e_ + 1 < n_experts:
                            wi_next = load_e(e_ + 1)
                        for j0 in range(jlo, jhi, 2):
                            expert_tiles(e_, wi, j0)
                    sc.end_guard()
            ykeys = list(dict.fromkeys(ystore_keys))
            for t in range(NT):
                for k in range(2):
                    yg = nxt("yg")

                    def g_fn(e, sem, t=t, k=k, yg=yg):
                        e.indirect_dma_start(out=Yg[yg][:, :], out_offset=None, in_=Ys_d[:, :],
                                             in_offset=bass.IndirectOffsetOnAxis(ap=slot01[:, t, k:k + 1], axis=0)).then_inc(sem, 16)
                    op("pool", g_fn, reads=ykeys + [("slot", t)], writes=[("yg", yg)], dma_key="yg%d" % yg, ndma=1)
                    op("dve", lambda e, t=t, k=k, yg=yg: e.scalar_tensor_tensor(x_sb[:, t, :], Yg[yg], g01[:, t, k:k + 1], x_sb[:, t, :],
                                                                                ALU.mult, ALU.add),
                       reads=[("yg", yg), ("x", t), ("g0", t), ("g1", t)], writes=[("x", t)])

        def barrier():
            evs = []
            for name, E in sc.eng.items():
                if E["count"] > 0:
                    evs.append((name, E["sem"], E["count"], name))
            for k, d in sc.dsem.items():
                evs.append(("d_" + str(k), d[0], d[1], "dma"))
            for name in ("pe", "act", "dve", "pool", "sp"):
                sc.wait_all(name, [ev for ev in evs if ev[0] != name])

        store_events = []
        for s in range(nseq):
            dma("sp", "xin", [(x_sb[:, 4 * g:4 * g + 4, :], x_d[s, 512 * g:512 * g + 512, :].rearrange("(t p) d -> p t d", p=128))
                              for g in range(4)],
                writes=[("x", t) for t in range(NT)])
            for l in range(depth):
                if do_attn:
                    barrier()
                    attn_phase(s, l)
                if do_moe:
                    barrier()
                    if sparse:
                        moe_phase_sparse(s, l)
                    else:
                        moe_phase(s, l)
            ev = dma("sp", "xout", [(y_d[s, 512 * g:512 * g + 512, :].rearrange("(t p) d -> p t d", p=128), x_sb[:, 4 * g:4 * g + 4, :])
                                    for g in range(4)],
                     reads=[("x", t) for t in range(NT)])
            store_events.append(ev)
        sc.wait_all("sp", store_events)
        sc.emit()
    nc._in_names = list(dr)
    return nc


_CACHE = {}


def kernel(**inputs):
    shared = _host_prep(inputs)
    x = np.ascontiguousarray(np.asarray(inputs["x"], dtype=np.float32))
    mem = np.ascontiguousarray(np.asarray(inputs["mem"], dtype=np.float32))
    if "nc" not in _CACHE:
        _CACHE["nc"] = build_nc()
    nc = _CACHE["nc"]
    in_maps = []
    for c in range(N_CORES):
        m = dict(shared)
        m["x"] = x[c * NSEQ:(c + 1) * NSEQ]
        m["mem"] = mem[c * NSEQ:(c + 1) * NSEQ]
        in_maps.append(m)
    res = run_bass_kernel_spmd(nc, in_maps, core_ids=list(range(N_CORES)))
    out = np.concatenate([np.asarray(r["y"]) for r in res.results], axis=0)
    return out.astype(np.float32)
```

```python
import contextlib
import numpy as np
import concourse.bass as bass
import concourse.mybir as mybir
from concourse.bass_utils import run_bass_kernel_spmd

F32 = mybir.dt.float32
BF16 = mybir.dt.bfloat16
AF = mybir.ActivationFunctionType
ALU = mybir.AluOpType
AX = mybir.AxisListType

D = 1024
S = 2048
NT = 16
MEM = 256
DEPTH = 2
NSEQ = 4
NE = 32
DE = 512
EPS = 1e-6
NEG = -30000.0
N_CORES = 8


class Sched:
    def __init__(self, nc, stack):
        self.nc = nc
        self.stack = stack
        self.eng = {}
        for name in ("pe", "act", "dve", "pool", "sp"):
            sem = stack.enter_context(nc.semaphore("s_" + name))
            self.eng[name] = dict(sem=sem, count=0, ops=[], seen={})
        self.res = {}
        self.dsem = {}
        self.guard = None
        self.gopened = set()

    def _need(self, eng, reads, writes):
        need = {}

        def add(ev, kind):
            sid, sem, val, src = ev
            if src == eng:
                if eng in ("pe", "sp"):
                    return
                if kind != "raw":
                    return
            if need.get(sid, (None, 0))[1] < val:
                need[sid] = (sem, val)

        for k in reads:
            r = self.res.get(k)
            if r and r["w"]:
                add(r["w"], "raw")
        for k in writes:
            r = self.res.get(k)
            if r:
                if r["w"]:
                    add(r["w"], "waw")
                for e in r["r"].values():
                    add(e, "war")
        return need

    def begin_guard(self, flag_ap, flag_key):
        self.guard = (flag_ap, flag_key)
        self.gopened = set()
        self.gsnap = {}

    def end_guard(self):
        for eng in self.gopened:
            self.eng[eng]["ops"].append(("gclose",))
            self.eng[eng]["seen"] = self.gsnap[eng]
        self.guard = None
        self.gopened = set()

    def op(self, eng, fn, reads=(), writes=(), dma_key=None, ndma=0):
        E = self.eng[eng]
        first_guarded = False
        if self.guard is not None and eng not in self.gopened:
            fneed = self._need(eng, [self.guard[1]], [])
            fw = []
            for sid, (sem, val) in fneed.items():
                if E["seen"].get(sid, 0) >= val:
                    continue
                E["seen"][sid] = val
                fw.append((sem, val))
            drain = [(E["sem"], E["count"])] if (E["count"] > 0 and eng != "sp") else []
            dtot = {k: (d[0], d[1]) for k, d in self.dsem.items()}
            E["ops"].append(("gopen", fw, self.guard[0], drain, dtot))
            self.gopened.add(eng)
            self.gsnap[eng] = dict(E["seen"])
            first_guarded = True
        need = self._need(eng, reads, writes)
        waits = []
        for sid, (sem, val) in need.items():
            if E["seen"].get(sid, 0) >= val:
                continue
            E["seen"][sid] = val
            waits.append((sem, val))
        if dma_key is None:
            E["count"] += 1
            ev = (eng, E["sem"], E["count"], eng)
            inc = E["sem"]
            rkey = eng
        else:
            d = self.dsem.get(dma_key)
            if d is None:
                sem = self.stack.enter_context(self.nc.semaphore("d_" + str(dma_key)))
                d = self.dsem[dma_key] = [sem, 0]
            d[1] += 16 * ndma
            ev = ("d_" + str(dma_key), d[0], d[1], "dma")
            inc = ("dma", d[0])
            rkey = "d_" + str(dma_key)
        E["ops"].append((waits, fn, inc, 16 * ndma))
        if first_guarded:
            self.res.setdefault(self.guard[1], dict(w=None, r={}))["r"]["g_" + eng] = ev
        for k in writes:
            self.res[k] = dict(w=ev, r={})
        for k in reads:
            self.res.setdefault(k, dict(w=None, r={}))["r"][rkey] = ev
        return ev

    def wait_all(self, eng, events):
        E = self.eng[eng]
        waits = []
        for sid, sem, val, src in events:
            if E["seen"].get(sid, 0) >= val:
                continue
            E["seen"][sid] = val
            waits.append((sem, val))
        E["ops"].append((waits, None, None, 0))

    def emit(self):
        nc = self.nc
        with nc.Block() as block:
            table = (("pe", block.tensor), ("act", block.scalar), ("dve", block.vector),
                     ("pool", block.gpsimd), ("sp", block.sync))
            for name, deco in table:
                ops = self.eng[name]["ops"]

                def body(e, ops=ops, name=name):
                    def real(rec):
                        waits, fn, inc, nd = rec
                        for sem, val in waits:
                            e.wait_ge(sem, val)
                        if fn is None:
                            return
                        if isinstance(inc, tuple):
                            fn(e, inc[1])
                        else:
                            fn(e).then_inc(inc, 1)

                    def ghost(rec):
                        waits, fn, inc, nd = rec
                        for sem, val in waits:
                            e.wait_ge(sem, val)
                        if fn is None:
                            return
                        if isinstance(inc, tuple):
                            e.sem_inc(inc[1], nd)
                        else:
                            e.sem_inc(inc, 1)

                    with e.register("gr_" + name) as greg:
                        i = 0
                        n = len(ops)
                        while i < n:
                            rec = ops[i]
                            if rec[0] == "gopen":
                                for sem, val in rec[1]:
                                    e.wait_ge(sem, val)
                                e.reg_load(greg, rec[2])
                                j = i + 1
                                blk = []
                                while ops[j][0] != "gclose":
                                    blk.append(ops[j])
                                    j += 1
                                with e.If_ne(greg, 0):
                                    for r in blk:
                                        real(r)
                                with e.Else():
                                    for sem, val in rec[3]:
                                        e.wait_ge(sem, val)
                                    nown = 0
                                    dadd = {}
                                    for r in blk:
                                        if r[1] is None:
                                            continue
                                        if isinstance(r[2], tuple):
                                            dadd[id(r[2][1])] = (r[2][1], dadd.get(id(r[2][1]), (None, 0))[1] + r[3])
                                        else:
                                            nown += 1
                                    for sem, add in dadd.values():
                                        for k, (dsem_h, tot) in rec[4].items():
                                            if dsem_h is sem and tot > 0:
                                                e.wait_ge(sem, tot)
                                        e.sem_inc(sem, add)
                                    if nown:
                                        e.sem_inc(self.eng[name]["sem"], nown)
                                i = j + 1
                                continue
                            real(rec)
                            i += 1

                deco(body)


def ssl(start, n, step):
    return slice(start, start + step * (n - 1) + 1, step)


def _alibi_slopes(n):
    return (2.0 ** (-8.0 * np.arange(1, n + 1) / n)).astype(np.float32)


def _bias_a():
    sl = _alibi_slopes(6)
    k = np.arange(128)[:, None]
    q = np.arange(128)[None, :]
    out = np.empty((6, 3, 128, 256), np.float32)
    for h in range(6):
        for p, dil in enumerate((1, 4, 16)):
            lo = np.where(k >= q, -sl[h] * dil * np.abs(k - q - 64).astype(np.float32), NEG)
            hi = np.where(k <= q, -sl[h] * dil * np.abs(k - q + 64).astype(np.float32), NEG)
            out[h, p, :, :128] = lo
            out[h, p, :, 128:] = hi
    return out


NA_VARIANTS = [(5, 5 + d) for d in (-2, -1, 0, 1, 2)] + \
              [(T, J) for T in (0, 1) for J in range(4)] + \
              [(T, J) for T in (14, 15) for J in range(12, 16)]


def na_variant(T, J):
    if 2 <= T <= 13:
        return J - T + 2
    if T < 2:
        return 5 + T * 4 + J
    return 13 + (T - 14) * 4 + (J - 12)


def na_keytiles(T):
    if T < 2:
        return list(range(4))
    if T > 13:
        return list(range(12, 16))
    return list(range(T - 2, T + 3))


def _bias_b(rpb):
    kl = np.arange(128)
    kr_off, kc = kl // 64, kl % 64
    qr_off, qc = kl // 64, kl % 64
    out = np.empty((DEPTH, 6, 128, len(NA_VARIANTS), 128), np.float32)
    for v, (T, J) in enumerate(NA_VARIANTS):
        r = (2 * T + qr_off)[None, :]
        keyrow = (2 * J + kr_off)[:, None]
        r0 = np.clip(r - 4, 0, 24)
        row_ok = (keyrow >= r0) & (keyrow < r0 + 8)
        c0 = np.clip(qc - 8, 0, 48)[None, :]
        col_ok = (kc[:, None] >= c0) & (kc[:, None] < c0 + 16)
        ok = row_ok & col_ok
        dr = np.clip(keyrow - r, -7, 7) + 7
        dc = np.clip(kc[:, None] - qc[None, :], -15, 15) + 15
        g = rpb[:, :, dr, dc]
        out[:, :, :, v, :] = np.where(ok[None, None], g, np.float32(NEG))
    return out


def _host_prep(inp):
    f = lambda a: np.ascontiguousarray(np.asarray(a, dtype=np.float32))
    w_in = f(inp["w_in"]).reshape(DEPTH, 8, 128, 20, 128).transpose(0, 3, 2, 1, 4)
    w_mem = f(inp["w_mem_kv"]).reshape(DEPTH, 8, 128, 4, 128).transpose(0, 3, 2, 1, 4)
    w_out = f(inp["w_out"]).reshape(DEPTH, 8, 128, 1024).transpose(0, 2, 1, 3)
    w_gate = f(inp["w_gate"]).reshape(DEPTH, NE, 8, 128, DE).transpose(0, 1, 3, 2, 4)
    w_up = f(inp["w_up"]).reshape(DEPTH, NE, 8, 128, DE).transpose(0, 1, 3, 2, 4)
    w_down = f(inp["w_down"]).reshape(DEPTH, NE, 4, 128, D).transpose(0, 1, 3, 2, 4)
    w_rt = np.concatenate([f(inp["w_group"]), f(inp["w_router"])], axis=2)
    w_rt = w_rt.reshape(DEPTH, 8, 128, 36).transpose(0, 2, 1, 3)
    b_rt = np.concatenate([f(inp["b_group"]), f(inp["b_router"])], axis=1)
    qkg = np.tile(f(inp["qk_gain"]), (1, 1, 2)).transpose(2, 0, 1).reshape(128, DEPTH * 6)
    og = f(inp["out_gain"]).reshape(DEPTH, 8, 128).transpose(2, 0, 1).reshape(128, DEPTH * 8)
    shared = dict(
        w_in=np.ascontiguousarray(w_in), w_mem=np.ascontiguousarray(w_mem),
        w_out=np.ascontiguousarray(w_out), w_gate=np.ascontiguousarray(w_gate),
        w_up=np.ascontiguousarray(w_up), w_down=np.ascontiguousarray(w_down),
        w_rt=np.ascontiguousarray(w_rt), b_rt=np.ascontiguousarray(b_rt),
        g_mix=f(inp["norm_mix"]), g_mem=f(inp["norm_mem"]), g_ffn=f(inp["norm_ffn"]),
        qkg=np.ascontiguousarray(qkg), og=np.ascontiguousarray(og),
        bias_a=_bias_a(), bias_b=_bias_b(f(inp["rpb"])),
        ident=np.eye(128, dtype=np.float32),
        triu=np.triu(np.ones((128, 128), np.float32), 1),
        eoff=np.tile((np.arange(NE, dtype=np.float32) * S)[None, :], (128, 1)),
        bones=np.kron(np.eye(2, dtype=np.float32), np.ones((64, 64), np.float32)),
    )
    return shared


def build_nc(nseq=NSEQ, depth=DEPTH, do_attn=True, do_moe=True, n_experts=NE, stop=None, sparse=True, guard=True):
    nc = bass.Bass("TRN2", target_bir_lowering=False)
    dr = {}

    def din(name, shape):
        dr[name] = nc.dram_tensor(name, list(shape), F32, kind="ExternalInput").ap()
        return dr[name]

    x_d = din("x", (nseq, S, D))
    mem_d = din("mem", (nseq, MEM, D))
    w_in_d = din("w_in", (DEPTH, 20, 128, 8, 128))
    w_mem_d = din("w_mem", (DEPTH, 4, 128, 8, 128))
    w_out_d = din("w_out", (DEPTH, 128, 8, 1024))
    if do_moe:
        w_gate_d = din("w_gate", (DEPTH, NE, 128, 8, DE))
        w_up_d = din("w_up", (DEPTH, NE, 128, 8, DE))
        w_down_d = din("w_down", (DEPTH, NE, 128, 4, D))
    w_rt_d = din("w_rt", (DEPTH, 128, 8, 36))
    b_rt_d = din("b_rt", (DEPTH, 36))
    g_mix_d = din("g_mix", (DEPTH, D))
    g_mem_d = din("g_mem", (DEPTH, D))
    g_ffn_d = din("g_ffn", (DEPTH, D))
    qkg_d = din("qkg", (128, DEPTH * 6))
    og_d = din("og", (128, DEPTH * 8))
    bias_a_d = din("bias_a", (6, 3, 128, 256))
    bias_b_d = din("bias_b", (DEPTH, 6, 128, 21, 128))
    ident_d = din("ident", (128, 128))
    triu_d = din("triu", (128, 128))
    eoff_d = din("eoff", (128, NE))
    I32 = mybir.dt.int32
    Hs_d = nc.dram_tensor("Hs_scr", [NE * S, D], BF16).ap()
    Ys_d = nc.dram_tensor("Ys_scr", [NE * S, D], F32).ap()
    bones_d = din("bones", (128, 128))
    y_d = nc.dram_tensor("y", [nseq, S, D], F32, kind="ExternalOutput").ap()

    stack = contextlib.ExitStack()
    with stack:
        def sb(name, shape, dt=F32):
            return stack.enter_context(nc.sbuf_tensor(name, list(shape), dt))

        UN = 43 * 1024
        U = sb("U", (128, UN), BF16)

        class Bump:
            def __init__(self):
                self.off = 0

            def alloc(self, shape, dt=F32):
                n = int(np.prod(shape[1:]))
                ne = n * 2 if dt == F32 else n
                ne = (ne + 1) // 2 * 2
                assert self.off + ne <= UN, ("union overflow", self.off, ne)
                v = U[:, self.off:self.off + ne]
                self.off += ne
                if dt == F32:
                    v = v.bitcast(F32)
                if len(shape) == 3:
                    v = v.rearrange("p (a b) -> p a b", a=shape[1])
                elif len(shape) == 4:
                    v = v.rearrange("p (a b c) -> p a b c", a=shape[1], b=shape[2])
                elif len(shape) == 5:
                    v = v.rearrange("p (a b c d) -> p a b c d", a=shape[1], b=shape[2], c=shape[3])
                return v

        x_sb = sb("x_sb", (128, NT, D))
        hT = sb("hT", (128, 8, S), BF16)
        sq = [sb("sq%d" % i, (128, 512), BF16) for i in range(2)]
        rs = [sb("rs%d" % i, (128, 512)) for i in range(2)]
        htok = [sb("htok%d" % i, (128, D), BF16) for i in range(2)]
        junk = sb("junk", (128, D), BF16)
        gbc = sb("gbc", (128, D))
        ssum = sb("ssum", (128, 32))
        rstd = sb("rstd", (128, 32))
        ident = sb("ident_sb", (128, 128), BF16)
        bones = sb("bones_sb", (128, 128), BF16)
        ones = sb("ones_sb", (128, 128), BF16)
        qkg = sb("qkg_sb", (128, DEPTH * 6))
        qkg8 = sb("qkg8_sb", (128, DEPTH * 6))
        og = sb("og_sb", (128, DEPTH * 8))
        Wr = sb("Wr", (128, 8, 36), BF16)
        brt = sb("brt", (128, 36))
        gates = sb("gates", (128, NT, NE))
        lg = sb("lg", (128, 36))
        rt_s = sb("rt_s", (128, 16))
        mx8 = sb("mx8", (128, 8))
        lmask = sb("lmask", (128, NE))
        eq1 = sb("eq1", (128, NE))
        eq2 = sb("eq2", (128, NE))
        triu = sb("triu_sb", (128, 128), BF16)
        eoff = sb("eoff_sb", (128, NE))
        Mall = sb("Mall", (128, NT, NE), BF16)
        slotf = sb("slotf", (128, NE))
        slot01f = sb("slot01f", (128, 2))
        slot01 = sb("slot01", (128, NT, 2), I32)
        g01 = sb("g01", (128, NT, 2))
        cntf = sb("cntf", (128, 2 * NE))
        flag_i = sb("flag_i", (128, 2 * NE), I32)
        ba = Bump()
        oT = ba.alloc((128, 3, S), BF16)
        acc = ba.alloc((128, S))
        qT = ba.alloc((128, S), BF16)
        kT = ba.alloc((128, S), BF16)
        vT = ba.alloc((128, S), BF16)
        VA = [ba.alloc((128, NT, 2, 128), BF16) for i in range(2)]
        Wt = [ba.alloc((128, 3, 8, 128), BF16) for i in range(1)]
        Wo = ba.alloc((128, 3, D), BF16)
        biasA = ba.alloc((128, 2, 3, 256), BF16)
        biasB = ba.alloc((128, 21, 128), BF16)
        PT = [ba.alloc((128, 512), BF16) for i in range(2)]
        mem_sb = acc.rearrange("p (a b) -> p a b", a=2)
        kmT = ba.alloc((128, 2, MEM), BF16)
        vmT = ba.alloc((128, MEM), BF16)
        VAM = ba.alloc((128, 2, 2, 2, 128), BF16)
        bm = Bump()
        Wg = [bm.alloc((128, 8, DE), BF16) for i in range(2)]
        Wu = [bm.alloc((128, 8, DE), BF16) for i in range(2)]
        Wd = [bm.alloc((128, 4, D), BF16) for i in range(2)]
        sg = [bm.alloc((128, 256 if sparse else 512), BF16) for i in range(2)]
        hid = [bm.alloc((128, 4, 256 if sparse else 512), BF16) for i in range(2)]
        if sparse:
            hTe = [bm.alloc((128, 8, 256), BF16) for i in range(2)]
            hs_tok = [bm.alloc((128, 2, D), BF16) for i in range(2)]
            yout = [bm.alloc((128, D)) for i in range(2)]
            Yg = [bm.alloc((128, D)) for i in range(2)]

        def ps(name, shape, dt=F32):
            return stack.enter_context(nc.psum_tensor(name, list(shape), dt))
        pA = [ps("pA%d" % i, (128, 512)) for i in range(3)]
        pTr = ps("pTr", (128, 8, 128), BF16)
        pY = [ps("pY%d" % i, (128, 1024)) for i in range(2)]
        B = [(pA[0], ("ps", 0)), (pA[1], ("ps", 1)), (pA[2], ("ps", 2))]
        BK_TR = ("ps", 3)
        Yk = [(("ps", 4), ("ps", 5)), (("ps", 6), ("ps", 7))]

        sc = Sched(nc, stack)
        op = sc.op

        def dma(queue, key, pairs, reads=(), writes=()):
            def fn(e, sem, pairs=pairs):
                for o, i in pairs:
                    e.dma_start(out=o, in_=i).then_inc(sem, 16)
            return op(queue, fn, reads=reads, writes=writes, dma_key=key, ndma=len(pairs))

        dma("pool", "cst", [(ident[:], ident_d), (bones[:], bones_d)], writes=["ident", "bones"])
        dma("sp", "cst2", [(qkg[:], qkg_d), (og[:], og_d), (eoff[:], eoff_d)], writes=["qkg", "og", "eoff"])
        dma("pool", "cst3", [(triu[:], triu_d)], writes=["triu"])
        op("pool", lambda e: e.memset(ones[:], 1.0), writes=["ones"])
        op("dve", lambda e: e.tensor_scalar_mul(qkg8[:], qkg[:], 0.125), reads=["qkg"], writes=["qkg8"])

        rr = dict(hte=0, hst=0, yo=0, yg=0, proj=0, st=0, pv=0, tr=0, y=0, pt=0, sq=0, rs=0, ht=0, wt=0, va=0, sg=0, hid=0, wm=0)

        def nxt(name, n=2):
            v = rr[name]
            rr[name] = (v + 1) % n
            return v

        def norm_to_hT(src, src_key, ntile, gain_d_row, dstT, dst_key):
            dma("sp", "gbc", [(gbc[:], gain_d_row.partition_broadcast(128))], writes=["gbc"])
            if stop == "n1":
                return
            for t in range(ntile):
                op("act", lambda e, t=t: e.activation(junk[:], src[:, t, :], AF.Square,
                                                      accum_out=ssum[:, t:t + 1]),
                   reads=[(src_key, t)], writes=["junk", ("ssum", t)])
                if stop == "n2":
                    continue
                op("act", lambda e, t=t: e.activation(rstd[:, t:t + 1], ssum[:, t:t + 1], AF.Ln,
                                                      bias=float(EPS), scale=1.0 / D),
                   reads=[("ssum", t)], writes=[("rstdq", t), ("rstd", t)])
                op("act", lambda e, t=t: e.activation(rstd[:, t:t + 1], rstd[:, t:t + 1], AF.Exp, scale=-0.5),
                   reads=[("rstdq", t)], writes=[("rstd", t)])
                if stop == "n3":
                    continue
                hb = nxt("ht")
                op("dve", lambda e, t=t, hb=hb: e.scalar_tensor_tensor(
                    htok[hb][:], src[:, t, :], rstd[:, t:t + 1], gbc[:], ALU.mult, ALU.mult),
                   reads=[(src_key, t), ("rstd", t), "gbc"], writes=[("htok", hb)])
                if stop == "n4":
                    continue

                def tr(e, hb=hb):
                    ins = None
                    for c in range(8):
                        ins = e.transpose(pTr[:, c, :], htok[hb][:, c * 128:(c + 1) * 128], ident[:])
                    return ins
                op("pe", tr, reads=[("htok", hb), "ident"], writes=[BK_TR])
                if stop == "n5":
                    continue
                op("act", lambda e, t=t: e.copy(dstT[:, :, t * 128:(t + 1) * 128], pTr[:]),
                   reads=[BK_TR], writes=[(dst_key, t)])

        def proj_chunk(W_ap, w_key, srcT, src_key, ntok, consume):
            nblk = (ntok + 511) // 512
            pend = None
            for b in range(nblk):
                n = min(512, ntok - b * 512)
                bi = nxt("proj")
                bank, bkey = B[bi]

                def mm(e, b=b, n=n, bank=bank):
                    ins = None
                    for kc in range(8):
                        ins = e.matmul(bank[:, 0:n], W_ap(kc), srcT[:, kc, b * 512:b * 512 + n],
                                       start=(kc == 0), stop=(kc == 7))
                    return ins
                tiles = [(src_key, t) for t in range(b * 4, b * 4 + (n + 127) // 128)]
                op("pe", mm, reads=[w_key] + tiles, writes=[bkey])
                if pend is not None:
                    consume(*pend)
                pend = (bank, bkey, b, n)
            if pend is not None:
                consume(*pend)

        def qk_consume(dst, dst_key, gcol_ap):
            def consume(bank, bkey, b, n):
                si = nxt("sq")
                op("act", lambda e: e.activation(sq[si][:, 0:n], bank[:, 0:n], AF.Square),
                   reads=[bkey], writes=[("sq", si)])
                sbank, skey = B[2]
                op("pe", lambda e: e.matmul(sbank[:, 0:n], bones[:], sq[si][:, 0:n], start=True, stop=True),
                   reads=[("sq", si), "bones"], writes=[skey])
                ri = nxt("rs")
                op("act", lambda e: e.activation(rs[ri][:, 0:n], sbank[:, 0:n], AF.Ln, bias=float(EPS), scale=1.0 / 64),
                   reads=[skey], writes=[("rsl", ri), ("rs", ri)])
                op("act", lambda e: e.activation(rs[ri][:, 0:n], rs[ri][:, 0:n], AF.Exp, scale=-0.5),
                   reads=[("rsl", ri)], writes=[("rs", ri)])
                op("dve", lambda e: e.scalar_tensor_tensor(dst[:, b * 512:b * 512 + n], bank[:, 0:n], gcol_ap,
                                                           rs[ri][:, 0:n], ALU.mult, ALU.mult),
                   reads=[bkey, ("rs", ri), "qkg", "qkg8"], writes=[(dst_key, b)])
            return consume

        def copy_consume(dst, dst_key):
            def consume(bank, bkey, b, n):
                op("act", lambda e: e.copy(dst[:, b * 512:b * 512 + n], bank[:, 0:n]),
                   reads=[bkey], writes=[(dst_key, b)])
            return consume

        def build_va(va, va_key, tile_tokens, src, src_keys):
            nt = len(tile_tokens)
            for g0 in range(0, nt, 8):
                g1 = min(nt, g0 + 8)

                def tr(e, g0=g0, g1=g1):
                    ins = None
                    for t in range(g0, g1):
                        ins = e.transpose(pTr[:, t - g0, :], src[:, tile_tokens[t]], ident[:])
                    return ins
                op("pe", tr, reads=list(src_keys) + ["ident"], writes=[BK_TR])
                op("dve", lambda e, g0=g0, g1=g1: e.tensor_copy(va[:, g0:g1, 0, 0:64], pTr[:, 0:g1 - g0, 0:64]),
                   reads=[BK_TR], writes=[va_key])
                op("dve", lambda e, g0=g0, g1=g1: e.tensor_copy(va[:, g0:g1, 1, 64:128], pTr[:, 0:g1 - g0, 64:128]),
                   reads=[BK_TR], writes=[va_key + ("b",)])

        def attention(qblocks, q_keys, k_keys, va_keys, bias_keys, acc_key):
            i = 0
            nq = len(qblocks)
            prev_stage2 = None
            while i < nq:
                cols = 0
                grp = []
                while i < nq and cols + qblocks[i]["n"] * len(qblocks[i]["items"]) <= 512:
                    grp.append(qblocks[i])
                    cols += qblocks[i]["n"] * len(qblocks[i]["items"])
                    i += 1
                    if len(grp) > 0 and i < nq and qblocks[i].get("flush_before"):
                        break
                assert grp, "qblock too large"
                sti = 1 - nxt("st")
                sbank, skey = (pY[0][:, sti * 512:(sti + 1) * 512], Yk[0][sti])
                pti = nxt("pt")

                def st_mm(e, grp=grp, sbank=sbank):
                    ins = None
                    c = 0
                    first = True
                    for qb in grp:
                        for (k_ap, v_ap, b_ap) in qb["items"]:
                            if b_ap is not None:
                                e.matmul(sbank[:, c:c + qb["n"]], ident[:], b_ap, start=first, stop=False,
                                         skip_group_check=True)
                                first = False
                            c += qb["n"]
                    c = 0
                    for qb in grp:
                        for (k_ap, v_ap, b_ap) in qb["items"]:
                            ins = e.matmul(sbank[:, c:c + qb["n"]], k_ap, qb["q"],
                                           start=(first and b_ap is None), stop=True, skip_group_check=True)
                            if b_ap is None:
                                first = False
                            c += qb["n"]
                    return ins
                op("pe", st_mm, reads=list(q_keys) + list(k_keys) + list(bias_keys) + ["ident"], writes=[skey])
                op("act", lambda e, cols=cols, sbank=sbank, pti=pti: e.activation(PT[pti][:, 0:cols], sbank[:, 0:cols], AF.Exp),
                   reads=[skey], writes=[("PT", pti)])
                def stage2(grp=grp, pti=pti):
                    pv_pending = []
                    c = 0
                    for qb in grp:
                        offs = []
                        for _ in qb["items"]:
                            offs.append(c)
                            c += qb["n"]
                        pv_pending.append((qb, pti, offs))
                    j = 0
                    while j < len(pv_pending):
                        tot = 0
                        piece = []
                        while j < len(pv_pending) and tot + pv_pending[j][0]["n"] <= 512:
                            piece.append(pv_pending[j])
                            tot += pv_pending[j][0]["n"]
                            j += 1
                        pvi = nxt("pv")
                        pbank, pkey = (pY[1][:, pvi * 512:(pvi + 1) * 512], Yk[1][pvi])

                        def pv_mm(e, piece=piece, pbank=pbank):
                            ins = None
                            c2 = 0
                            for (qb, pti2, offs) in piece:
                                for ii, (k_ap, v_ap, b_ap) in enumerate(qb["items"]):
                                    ins = e.matmul(pbank[:, c2:c2 + qb["n"]], v_ap,
                                                   PT[pti2][:, offs[ii]:offs[ii] + qb["n"]],
                                                   start=(ii == 0), stop=(ii == len(qb["items"]) - 1),
                                                   skip_group_check=True)
                                c2 += qb["n"]
                            return ins
                        op("pe", pv_mm, reads=[("PT", piece[0][1])] + list(va_keys), writes=[pkey])
                        c2 = 0
                        for (qb, pti2, offs) in piece:
                            n = qb["n"]
                            if qb["add"]:
                                op("dve", lambda e, qb=qb, c2=c2, n=n, pbank=pbank: e.tensor_tensor(
                                    qb["dst"], pbank[:, c2:c2 + n], qb["dst"], ALU.add),
                                   reads=[pkey, acc_key], writes=[acc_key])
                            else:
                                op("dve", lambda e, qb=qb, c2=c2, n=n, pbank=pbank: e.tensor_copy(
                                    qb["dst"], pbank[:, c2:c2 + n]),
                                   reads=[pkey], writes=[acc_key])
                            c2 += n
                    pv_pending = []

                if prev_stage2 is not None:
                    prev_stage2()
                prev_stage2 = stage2

            if prev_stage2 is not None:
                prev_stage2()

        def head_epilogue(hh, chunk_slot, acc_key, ntok=S):
            u = slice(0, 64) if hh == 0 else slice(64, 128)
            dn = slice(64, 128) if hh == 0 else slice(0, 64)
            for b in range(ntok // 512):
                cs = slice(b * 512, (b + 1) * 512)
                ri = nxt("rs")
                op("pool", lambda e, ri=ri, cs=cs: e.tensor_copy(rs[ri][u, :], acc[dn, cs]),
                   reads=[acc_key], writes=[("rsl", ri), ("rs", ri)])
                op("dve", lambda e, ri=ri: e.reciprocal(rs[ri][u, :], rs[ri][u, :]),
                   reads=[("rsl", ri)], writes=[("rs", ri)])
                op("dve", lambda e, ri=ri, cs=cs: e.tensor_tensor(oT[u, chunk_slot, cs], acc[u, cs], rs[ri][u, :], ALU.mult),
                   reads=[acc_key, ("rs", ri)], writes=[("oT", chunk_slot, hh)])

        def group_finalize(l, chunks, width):
            ng = len(chunks)
            dma("pool", "Wo", [(Wo[:, 0:ng, :], w_out_d[l, :, chunks[0]:chunks[0] + ng, :])], writes=["Wo"])
            for b in range(4):
                sbank, skey = B[2]
                for ci in range(ng):
                    si = nxt("sq")
                    op("act", lambda e, ci=ci, si=si, b=b: e.activation(sq[si][:], oT[:, ci, b * 512:(b + 1) * 512], AF.Square),
                       reads=[("oT", ci, 0), ("oT", ci, 1)], writes=[("sq", si)])
                    op("pe", lambda e, ci=ci, si=si: e.matmul(sbank[:], ones[:], sq[si][:], start=(ci == 0), stop=(ci == ng - 1)),
                       reads=[("sq", si), "ones"], writes=[skey])
                ri = nxt("rs")
                op("act", lambda e, ri=ri: e.activation(rs[ri][:], sbank[:], AF.Ln, bias=float(EPS), scale=1.0 / width),
                   reads=[skey], writes=[("rsl", ri), ("rs", ri)])
                op("act", lambda e, ri=ri: e.activation(rs[ri][:], rs[ri][:], AF.Exp, scale=-0.5),
                   reads=[("rsl", ri)], writes=[("rs", ri)])
                for ci in range(ng):
                    gcol = og[:, l * 8 + chunks[ci]:l * 8 + chunks[ci] + 1]
                    op("dve", lambda e, ci=ci, gcol=gcol, ri=ri, b=b: e.scalar_tensor_tensor(
                        oT[:, ci, b * 512:(b + 1) * 512], oT[:, ci, b * 512:(b + 1) * 512], gcol, rs[ri][:],
                        ALU.mult, ALU.mult),
                       reads=[("oT", ci, 0), ("oT", ci, 1), ("rs", ri), "og"], writes=[("mix", ci, b)])
            for t in range(NT):
                yi = nxt("y")

                def mm(e, t=t, yi=yi):
                    ins = None
                    for half in range(2):
                        for ci in range(ng):
                            ins = e.matmul(pY[yi][:, half * 512:(half + 1) * 512], oT[:, ci, t * 128:(t + 1) * 128],
                                           Wo[:, ci, half * 512:(half + 1) * 512], start=(ci == 0), stop=(ci == ng - 1))
                    return ins
                op("pe", mm, reads=[("mix", ci, t // 4) for ci in range(ng)] + ["Wo"], writes=list(Yk[yi]))
                op("dve", lambda e, t=t, yi=yi: e.tensor_tensor(x_sb[:, t, :], pY[yi][:], x_sb[:, t, :], ALU.add),
                   reads=list(Yk[yi]) + [("x", t)], writes=[("x", t)])
            for ci in range(ng):
                for hh in range(2):
                    sc.res[("oT", ci, hh)] = sc.res[("mix", ci, 3)]

        def load_pair_w(l, chunk_ids):
            wi = nxt("wt", len(Wt))
            dma("pool", "Wt%d" % wi, [(Wt[wi][:, j, :, :], w_in_d[l, cid]) for j, cid in enumerate(chunk_ids)],
                writes=[("Wt", wi)])
            return wi

        def attn_phase(s, l):
            for i in range(2):
                op("pool", lambda e, i=i: e.memset(VA[i], 1.0), writes=[("VA", i), ("VA", i, "b")])
            op("pool", lambda e: e.memset(VAM, 1.0), writes=["VAM", ("VAM", "b")])
            if stop == "memset":
                return
            mem_path(s, l)
            if stop == "mem" or (stop and stop[0] in "nm" and stop != "norm"):
                return
            norm_to_hT(x_sb, "x", NT, g_mix_d[l:l + 1, :], hT, "hT")
            if stop == "norm":
                return
            hkeys = [("hT", t) for t in range(NT)]
            for grp_name in ("A", "B", "M"):
                if grp_name == "M":
                    npair = 2
                else:
                    npair = 3
                for pr in range(npair):
                    if grp_name == "A":
                        cids = [pr, 3 + pr, 6 + pr]
                        gi = (0, 1)
                    elif grp_name == "B":
                        cids = [9 + pr, 12 + pr, 15 + pr]
                        gi = (2, 3)
                    else:
                        cids = [18 + pr]
                        gi = (4, 5)
                    wi = load_pair_w(l, cids)
                    proj_chunk(lambda kc, wi=wi: Wt[wi][:, 0, kc, :], ("Wt", wi), hT, "hT", S,
                               qk_consume(qT, "qT", qkg8[:, l * 6 + gi[0]:l * 6 + gi[0] + 1]))
                    qkeys = [("qT", b) for b in range(4)]
                    if grp_name != "M":
                        proj_chunk(lambda kc, wi=wi: Wt[wi][:, 1, kc, :], ("Wt", wi), hT, "hT", S,
                                   qk_consume(kT, "kT", qkg[:, l * 6 + gi[1]:l * 6 + gi[1] + 1]))
                        proj_chunk(lambda kc, wi=wi: Wt[wi][:, 2, kc, :], ("Wt", wi), hT, "hT", S,
                                   copy_consume(vT, "vT"))
                        kkeys = [("kT", b) for b in range(4)]
                        vkeys = [("vT", b) for b in range(4)]
                        hb = 0 if grp_name == "A" else 1
                        if grp_name == "A":
                            dma("pool", "biasA", [(biasA[:, hh, :, :], bias_a_d[2 * pr + hh].rearrange("p k q -> k p q"))
                                                  for hh in range(2)], writes=["biasA"])
                    if grp_name == "A":
                        for hh in range(2):
                            pass
                        accs = {}
                        for hh in range(2):
                            hs = slice(64 * hh, 64 * hh + 64)
                            for p, dil in enumerate((1, 4, 16)):
                                vi = p % 2 if hh == 0 else None
                        for hh in range(2):
                            hs = slice(64 * hh, 64 * hh + 64)
                            for p, dil in enumerate((1, 4, 16)):
                                L = S // dil
                                nkt = L // 128
                                vi = nxt("va")
                                va = VA[vi]
                                toks = [ssl(dil * 128 * j + r, 128, dil)
                                        for r in range(dil) for j in range(nkt)]
                                if hh == 0 or True:
                                    build_va(va, ("VA", vi), toks, vT, vkeys)
                                qbs = []
                                for r in range(dil):
                                    for i in range(nkt + 1):
                                        lo_l = 128 * i - 64
                                        q0 = max(lo_l, 0)
                                        q1 = min(lo_l + 128, L)
                                        n = q1 - q0
                                        qo = q0 - lo_l
                                        items = []
                                        if i >= 1:
                                            j = i - 1
                                            ksl = ssl(dil * 128 * j + r, 128, dil)
                                            items.append((kT[hs, ksl], va[:, r * nkt + j, hh, :],
                                                          biasA[:, hh, p, qo:qo + n]))
                                        if i <= nkt - 1:
                                            j = i
                                            ksl = ssl(dil * 128 * j + r, 128, dil)
                                            items.append((kT[hs, ksl], va[:, r * nkt + j, hh, :],
                                                          biasA[:, hh, p, 128 + qo:128 + qo + n]))
                                        qsl = ssl(dil * q0 + r, n, dil)
                                        qbs.append(dict(q=qT[hs, qsl], n=n, items=items, dst=acc[:, qsl],
                                                        add=(p > 0)))
                                attention(qbs, qkeys, kkeys, [("VA", vi), ("VA", vi, "b")], ["biasA"], "acc")
                            head_epilogue(hh, pr, "acc")
                    elif grp_name == "B":
                        vi = nxt("va")
                        va = VA[vi]
                        toks = [slice(128 * j, 128 * j + 128) for j in range(NT)]
                        build_va(va, ("VA", vi), toks, vT, vkeys)
                        for hh in range(2):
                            hs = slice(64 * hh, 64 * hh + 64)
                            dma("pool", "biasB", [(biasB, bias_b_d[l, 2 * pr + hh])], writes=["biasB"])
                            qbs = []
                            for T in range(NT):
                                items = []
                                for J in na_keytiles(T):
                                    v = na_variant(T, J)
                                    items.append((kT[hs, 128 * J:128 * J + 128], va[:, J, hh, :], biasB[:, v, :]))
                                qsl = slice(128 * T, 128 * T + 128)
                                qbs.append(dict(q=qT[hs, qsl], n=128, items=items[:4], dst=acc[:, qsl], add=False))
                                if len(items) > 4:
                                    qbs.append(dict(q=qT[hs, qsl], n=128, items=items[4:], dst=acc[:, qsl], add=True))
                            attention(qbs, qkeys, kkeys, [("VA", vi), ("VA", vi, "b")], ["biasB"], "acc")
                            head_epilogue(hh, pr, "acc")
                    else:
                        for hh in range(2):
                            hs = slice(64 * hh, 64 * hh + 64)
                            qbs = []
                            for b in range(4):
                                qsl = slice(512 * b, 512 * b + 512)
                                items = [(kmT[hs, pr, 128 * j:128 * j + 128], VAM[:, pr, j, hh, :], None) for j in range(2)]
                                qbs.append(dict(q=qT[hs, qsl], n=512, items=items[:1], dst=acc[:, qsl], add=False))
                                qbs.append(dict(q=qT[hs, qsl], n=512, items=items[1:], dst=acc[:, qsl], add=True))
                            attention(qbs, qkeys, [("kmT", pr)], ["VAM", ("VAM", "b")], [], "acc")
                            head_epilogue(hh, pr, "acc")
                if stop == grp_name + "attn":
                    return
                if grp_name == "A":
                    group_finalize(l, [0, 1, 2], 384)
                elif grp_name == "B":
                    group_finalize(l, [3, 4, 5], 384)
                else:
                    group_finalize(l, [6, 7], 256)
                if stop == grp_name:
                    return

        def mem_path(s, l):
            dma("sp", "mem", [(mem_sb, mem_d[s].rearrange("(t p) d -> p t d", p=128))],
                reads=[], writes=[("mem", 0), ("mem", 1), "acc"])
            norm_to_hT(mem_sb, "mem", 2, g_mem_d[l:l + 1, :], hT, "hT")
            if stop and stop.startswith("n"):
                return
            rd = {}
            for t in range(2):
                for k, v in sc.res[("mem", t)]["r"].items():
                    if k not in rd or rd[k][2] < v[2]:
                        rd[k] = v
            sc.res["acc"] = dict(w=None, r=rd)
            for j in range(4):
                if stop == "m0" or (stop == "m1" and j >= 1) or (stop == "m2" and j >= 2) or (stop == "m3" and j >= 3):
                    return
                wi = nxt("wt", len(Wt))
                dma("pool", "Wt%d" % wi, [(Wt[wi][:, 0, :, :], w_mem_d[l, j])], writes=[("Wt", wi)])
                if j < 2:
                    def consume(bank, bkey, b, n, j=j):
                        qk_consume(kmT[:, j, :], ("kmTb", j), qkg[:, l * 6 + 5:l * 6 + 6])(bank, bkey, b, n)
                        sc.res[("kmT", j)] = sc.res[(("kmTb", j), 0)]
                    proj_chunk(lambda kc, wi=wi: Wt[wi][:, 0, kc, :], ("Wt", wi), hT, "hT", MEM, consume)
                else:
                    proj_chunk(lambda kc, wi=wi: Wt[wi][:, 0, kc, :], ("Wt", wi), hT, "hT", MEM,
                               copy_consume(vmT, "vmT"))
                    pr = j - 2
                    toks = [slice(0, 128), slice(128, 256)]

                    def tr(e):
                        ins = None
                        for t in range(2):
                            ins = e.transpose(pTr[:, t, :], vmT[:, toks[t]], ident[:])
                        return ins
                    op("pe", tr, reads=[("vmT", 0), "ident"], writes=[BK_TR])
                    op("dve", lambda e, pr=pr: e.tensor_copy(VAM[:, pr, :, 0, 0:64], pTr[:, 0:2, 0:64]),
                       reads=[BK_TR], writes=["VAM"])
                    op("dve", lambda e, pr=pr: e.tensor_copy(VAM[:, pr, :, 1, 64:128], pTr[:, 0:2, 64:128]),
                       reads=[BK_TR], writes=[("VAM", "b")])

        def moe_phase(s, l):
            norm_to_hT(x_sb, "x", NT, g_ffn_d[l:l + 1, :], hT, "hT")
            dma("pool", "Wr", [(Wr[:], w_rt_d[l])], writes=["Wr"])
            dma("sp", "brt", [(brt[:], b_rt_d[l:l + 1, :].partition_broadcast(128))], writes=["brt"])
            for t in range(NT):
                bank, bkey = B[2]

                def mm(e, t=t):
                    ins = None
                    for kc in range(8):
                        ins = e.matmul(bank[:, 0:36], hT[:, kc, t * 128:(t + 1) * 128], Wr[:, kc, :],
                                       start=(kc == 0), stop=(kc == 7))
                    return ins
                op("pe", mm, reads=[("hT", t), "Wr"], writes=[bkey])
                op("dve", lambda e: e.tensor_tensor(lg[:], bank[:, 0:36], brt[:], ALU.add),
                   reads=[bkey, "brt"], writes=["lg"])
                op("dve", lambda e: e.reduce_max(rt_s[:, 0:1], lg[:, 0:4], AX.X), reads=["lg"], writes=["rt0"])
                op("dve", lambda e: e.tensor_scalar_mul(rt_s[:, 1:2], rt_s[:, 0:1], -1.0), reads=["rt0"], writes=["rt1"])
                op("act", lambda e: e.activation(junk[:, 0:4], lg[:, 0:4], AF.Exp, bias=rt_s[:, 1:2],
                                                 accum_out=rt_s[:, 2:3]),
                   reads=["lg", "rt1"], writes=["junk", "rt2"])
                op("dve", lambda e: e.reciprocal(rt_s[:, 3:4], rt_s[:, 2:3]), reads=["rt2"], writes=["rt3"])
                op("dve", lambda e: e.tensor_scalar(rt_s[:, 4:8], lg[:, 0:4], rt_s[:, 0:1], NEG * 10, ALU.is_lt, ALU.mult),
                   reads=["lg", "rt0"], writes=["rt4"])
                for g in range(4):
                    op("dve", lambda e, g=g: e.tensor_scalar_add(lmask[:, g * 8:(g + 1) * 8], lg[:, 4 + g * 8:12 + g * 8],
                                                                 rt_s[:, 4 + g:5 + g]),
                       reads=["lg", "rt4"], writes=[("lmask", g)])
                lm = [("lmask", g) for g in range(4)]
                op("dve", lambda e: e.reduce_max(mx8[:, 0:1], lmask[:], AX.X), reads=lm, writes=["mx8a"])
                op("dve", lambda e: e.tensor_scalar(eq2[:], lmask[:], mx8[:, 0:1], NEG * 10, ALU.is_equal, ALU.mult),
                   reads=lm + ["mx8a"], writes=["eq2"])
                op("dve", lambda e: e.tensor_tensor(eq1[:], lmask[:], eq2[:], ALU.add), reads=lm + ["eq2"], writes=["eq1"])
                op("dve", lambda e: e.reduce_max(mx8[:, 1:2], eq1[:], AX.X), reads=["eq1"], writes=["mx8"])
                op("dve", lambda e: e.tensor_tensor(rt_s[:, 8:9], mx8[:, 1:2], mx8[:, 0:1], ALU.subtract),
                   reads=["mx8", "mx8a"], writes=["rt8"])
                op("act", lambda e: e.activation(rt_s[:, 9:10], rt_s[:, 8:9], AF.Exp), reads=["rt8"], writes=["rt9"])
                op("dve", lambda e: e.tensor_scalar_add(rt_s[:, 9:10], rt_s[:, 9:10], 1.0), reads=["rt9"], writes=["rt9"])
                op("dve", lambda e: e.reciprocal(rt_s[:, 12:13], rt_s[:, 9:10]), reads=["rt9"], writes=["rt12"])
                op("dve", lambda e: e.tensor_tensor(rt_s[:, 10:11], rt_s[:, 3:4], rt_s[:, 12:13], ALU.mult),
                   reads=["rt12", "rt3"], writes=["rt10"])
                op("dve", lambda e: e.tensor_tensor(rt_s[:, 11:12], rt_s[:, 3:4], rt_s[:, 10:11], ALU.subtract),
                   reads=["rt10", "rt3"], writes=["rt11"])
                op("dve", lambda e: e.tensor_scalar(eq1[:], lmask[:], mx8[:, 0:1], rt_s[:, 10:11], ALU.is_equal, ALU.mult),
                   reads=lm + ["mx8", "mx8a", "rt10"], writes=["eq1"])
                op("dve", lambda e: e.tensor_scalar(eq2[:], lmask[:], mx8[:, 1:2], rt_s[:, 11:12], ALU.is_equal, ALU.mult),
                   reads=lm + ["mx8", "rt11"], writes=["eq2"])
                op("dve", lambda e, t=t: e.tensor_tensor(gates[:, t, :], eq1[:], eq2[:], ALU.add),
                   reads=["eq1", "eq2"], writes=[("gates", t)])
            def load_e(e_):
                wi = nxt("wm")
                dma("pool", "We%d" % wi, [(Wg[wi][:], w_gate_d[l, e_]), (Wu[wi][:], w_up_d[l, e_]),
                                          (Wd[wi][:], w_down_d[l, e_])], writes=[("We", wi)])
                return wi
            wi_next = load_e(0)
            for e_ in range(n_experts):
                wi = wi_next
                if e_ + 1 < n_experts:
                    wi_next = load_e(e_ + 1)
                for b in range(4):
                    hi_ = nxt("hid")
                    for dc in range(4):
                        gb = nxt("proj")
                        gbank, gkey = B[gb]
                        ubank, ukey = B[2] if False else (None, None)
                        ui = nxt("st")
                        ubank, ukey = (pY[0][:, ui * 512:(ui + 1) * 512], Yk[0][ui])

                        def mmg(e, wi=wi, b=b, dc=dc, gbank=gbank):
                            ins = None
                            for kc in range(8):
                                ins = e.matmul(gbank[:], Wg[wi][:, kc, dc * 128:(dc + 1) * 128], hT[:, kc, b * 512:(b + 1) * 512],
                                               start=(kc == 0), stop=(kc == 7))
                            return ins

                        def mmu(e, wi=wi, b=b, dc=dc, ubank=ubank):
                            ins = None
                            for kc in range(8):
                                ins = e.matmul(ubank, Wu[wi][:, kc, dc * 128:(dc + 1) * 128], hT[:, kc, b * 512:(b + 1) * 512],
                                               start=(kc == 0), stop=(kc == 7))
                            return ins
                        hk = [("hT", t) for t in range(4 * b, 4 * b + 4)]
                        op("pe", mmg, reads=[("We", wi)] + hk, writes=[gkey])
                        op("pe", mmu, reads=[("We", wi)] + hk, writes=[ukey])
                        si = nxt("sg")
                        op("act", lambda e, si=si, gbank=gbank: e.activation(sg[si][:], gbank[:], AF.Silu),
                           reads=[gkey], writes=[("sg", si)])
                        op("dve", lambda e, si=si, ubank=ubank, hi_=hi_, dc=dc: e.tensor_tensor(
                            hid[hi_][:, dc, :], ubank, sg[si][:], ALU.mult),
                           reads=[ukey, ("sg", si)], writes=[("hid", hi_, dc)])
                    for tt in range(4):
                        t = 4 * b + tt
                        yi = 1

                        def mmd(e, wi=wi, hi_=hi_, tt=tt):
                            ins = None
                            for half in range(2):
                                for dc in range(4):
                                    ins = e.matmul(pY[1][:, half * 512:(half + 1) * 512], hid[hi_][:, dc, tt * 128:(tt + 1) * 128],
                                                   Wd[wi][:, dc, half * 512:(half + 1) * 512], start=(dc == 0), stop=(dc == 3))
                            return ins
                        op("pe", mmd, reads=[("We", wi)] + [("hid", hi_, dc) for dc in range(4)], writes=list(Yk[1]))
                        op("dve", lambda e, t=t, e_=e_: e.scalar_tensor_tensor(
                            x_sb[:, t, :], pY[1][:], gates[:, t, e_:e_ + 1], x_sb[:, t, :], ALU.mult, ALU.add),
                           reads=list(Yk[1]) + [("x", t), ("gates", t)], writes=[("x", t)])

        def moe_phase_sparse(s, l):
            dma("pool", "Wr", [(Wr[:], w_rt_d[l])], writes=["Wr"])
            dma("sp", "brt", [(brt[:], b_rt_d[l:l + 1, :].partition_broadcast(128))], writes=["brt"])
            dma("sp", "gbc", [(gbc[:], g_ffn_d[l:l + 1, :].partition_broadcast(128))], writes=["gbc"])

            def load_e(e_):
                wi = nxt("wm")
                dma("pool", "We%d" % wi, [(Wg[wi][:], w_gate_d[l, e_]), (Wu[wi][:], w_up_d[l, e_]),
                                          (Wd[wi][:], w_down_d[l, e_])], writes=[("We", wi)])
                return wi
            wi_next = load_e(0)
            scat_keys = []
            for t in range(NT):
                op("act", lambda e, t=t: e.activation(junk[:], x_sb[:, t, :], AF.Square, accum_out=ssum[:, t:t + 1]),
                   reads=[("x", t)], writes=["junk", ("ssum", t)])
                op("act", lambda e, t=t: e.activation(rstd[:, t:t + 1], ssum[:, t:t + 1], AF.Ln, bias=float(EPS), scale=1.0 / D),
                   reads=[("ssum", t)], writes=[("rstdq", t), ("rstd", t)])
                op("act", lambda e, t=t: e.activation(rstd[:, t:t + 1], rstd[:, t:t + 1], AF.Exp, scale=-0.5),
                   reads=[("rstdq", t)], writes=[("rstd", t)])
                hb = nxt("ht")
                op("dve", lambda e, t=t, hb=hb: e.scalar_tensor_tensor(htok[hb][:], x_sb[:, t, :], rstd[:, t:t + 1], gbc[:], ALU.mult, ALU.mult),
                   reads=[("x", t), ("rstd", t), "gbc"], writes=[("htok", hb)])

                def tr(e, hb=hb):
                    ins = None
                    for c in range(8):
                        ins = e.transpose(pTr[:, c, :], htok[hb][:, c * 128:(c + 1) * 128], ident[:])
                    return ins
                op("pe", tr, reads=[("htok", hb), "ident"], writes=[BK_TR])
                op("act", lambda e, t=t: e.copy(hT[:, :, t * 128:(t + 1) * 128], pTr[:]), reads=[BK_TR], writes=[("hT", t)])
                bank, bkey = B[2]

                def mm(e, t=t):
                    ins = None
                    for kc in range(8):
                        ins = e.matmul(bank[:, 0:36], hT[:, kc, t * 128:(t + 1) * 128], Wr[:, kc, :], start=(kc == 0), stop=(kc == 7))
                    return ins
                op("pe", mm, reads=[("hT", t), "Wr"], writes=[bkey])
                op("dve", lambda e: e.tensor_tensor(lg[:], bank[:, 0:36], brt[:], ALU.add), reads=[bkey, "brt"], writes=["lg"])
                op("dve", lambda e: e.reduce_max(rt_s[:, 0:1], lg[:, 0:4], AX.X), reads=["lg"], writes=["rt0"])
                op("dve", lambda e: e.tensor_scalar_mul(rt_s[:, 1:2], rt_s[:, 0:1], -1.0), reads=["rt0"], writes=["rt1"])
                op("act", lambda e: e.activation(junk[:, 0:4], lg[:, 0:4], AF.Exp, bias=rt_s[:, 1:2], accum_out=rt_s[:, 2:3]),
                   reads=["lg", "rt1"], writes=["junk", "rt2"])
                op("dve", lambda e: e.reciprocal(rt_s[:, 3:4], rt_s[:, 2:3]), reads=["rt2"], writes=["rt3"])
                op("dve", lambda e: e.tensor_scalar(rt_s[:, 4:8], lg[:, 0:4], rt_s[:, 0:1], NEG * 10, ALU.is_lt, ALU.mult),
                   reads=["lg", "rt0"], writes=["rt4"])
                for g in range(4):
                    op("dve", lambda e, g=g: e.tensor_scalar_add(lmask[:, g * 8:(g + 1) * 8], lg[:, 4 + g * 8:12 + g * 8], rt_s[:, 4 + g:5 + g]),
                       reads=["lg", "rt4"], writes=[("lmask", g)])
                lm = [("lmask", g) for g in range(4)]
                op("dve", lambda e: e.reduce_max(mx8[:, 0:1], lmask[:], AX.X), reads=lm, writes=["mx8a"])
                op("dve", lambda e: e.tensor_scalar(eq1[:], lmask[:], mx8[:, 0:1], None, ALU.is_equal), reads=lm + ["mx8a"], writes=["eq1"])
                op("dve", lambda e: e.scalar_tensor_tensor(lmask[:], eq1[:], NEG * 10, lmask[:], ALU.mult, ALU.add),
                   reads=lm + ["eq1"], writes=lm)
                op("dve", lambda e: e.reduce_max(mx8[:, 1:2], lmask[:], AX.X), reads=lm, writes=["mx8"])
                op("dve", lambda e: e.tensor_scalar(eq2[:], lmask[:], mx8[:, 1:2], None, ALU.is_equal), reads=lm + ["mx8"], writes=["eq2"])
                op("dve", lambda e: e.tensor_tensor(rt_s[:, 8:9], mx8[:, 1:2], mx8[:, 0:1], ALU.subtract), reads=["mx8", "mx8a"], writes=["rt8"])
                op("act", lambda e: e.activation(rt_s[:, 9:10], rt_s[:, 8:9], AF.Exp), reads=["rt8"], writes=["rt9"])
                op("dve", lambda e: e.tensor_scalar_add(rt_s[:, 9:10], rt_s[:, 9:10], 1.0), reads=["rt9"], writes=["rt9"])
                op("dve", lambda e: e.reciprocal(rt_s[:, 12:13], rt_s[:, 9:10]), reads=["rt9"], writes=["rt12"])
                op("dve", lambda e, t=t: e.tensor_tensor(g01[:, t, 0:1], rt_s[:, 3:4], rt_s[:, 12:13], ALU.mult),
                   reads=["rt12", "rt3"], writes=[("g0", t)])
                op("dve", lambda e, t=t: e.tensor_tensor(g01[:, t, 1:2], rt_s[:, 3:4], g01[:, t, 0:1], ALU.subtract),
                   reads=[("g0", t), "rt3"], writes=[("g1", t)])
                op("dve", lambda e, t=t: e.tensor_tensor(Mall[:, t, :], eq1[:], eq2[:], ALU.add), reads=["eq1", "eq2"], writes=[("M", t)])
                def rk(e, t=t):
                    ins = None
                    for tp in range(t):
                        ins = e.matmul(bank[:, 64:64 + NE], ones[:], Mall[:, tp, :], start=(tp == 0), stop=False)
                    ins = e.matmul(bank[:, 64:64 + NE], triu[:], Mall[:, t, :], start=(t == 0), stop=True)
                    return ins
                op("pe", rk, reads=[("M", tp) for tp in range(t + 1)] + ["ones", "triu", "lg"], writes=[bkey])
                op("dve", lambda e: e.tensor_tensor(slotf[:], bank[:, 64:64 + NE], eoff[:], ALU.add), reads=[bkey, "eoff"], writes=["slotf"])
                op("dve", lambda e: e.tensor_tensor(eq1[:], eq1[:], slotf[:], ALU.mult), reads=["eq1", "slotf", ("M", t)], writes=["eq1"])
                op("dve", lambda e: e.tensor_tensor(eq2[:], eq2[:], slotf[:], ALU.mult), reads=["eq2", "slotf", ("M", t)], writes=["eq2"])
                op("dve", lambda e: e.reduce_sum(slot01f[:, 0:1], eq1[:], AX.X), reads=["eq1"], writes=["s0f"])
                op("dve", lambda e: e.reduce_sum(slot01f[:, 1:2], eq2[:], AX.X), reads=["eq2"], writes=["s1f"])
                op("dve", lambda e, t=t: e.tensor_copy(slot01[:, t, :], slot01f[:]), reads=["s0f", "s1f"], writes=[("slot", t)])
                for k in range(2):
                    def sc_fn(e, sem, t=t, k=k, hb=hb):
                        e.indirect_dma_start(out=Hs_d[:, :], out_offset=bass.IndirectOffsetOnAxis(ap=slot01[:, t, k:k + 1], axis=0),
                                             in_=htok[hb][:, :], in_offset=None).then_inc(sem, 16)
                    op("pool", sc_fn, reads=[("slot", t), ("htok", hb)], writes=[("Hs", t, k)], dma_key="hs%d" % hb, ndma=1)
                    scat_keys.append(("Hs", t, k))
            bank, bkey = B[2]

            def cntmm(e):
                ins = None
                for tp in range(NT):
                    ins = e.matmul(bank[:, 128:128 + NE], ones[:], Mall[:, tp, :], start=(tp == 0), stop=(tp == NT - 1))
                return ins
            op("pe", cntmm, reads=[("M", tp) for tp in range(NT)] + ["ones", "slotf"], writes=[bkey])
            op("dve", lambda e: e.tensor_scalar(cntf[:, 0:NE], bank[:, 128:128 + NE], 256.5, None, ALU.is_gt), reads=[bkey], writes=["cntf"])
            op("dve", lambda e: e.tensor_scalar(cntf[:, NE + 2:2 * NE], bank[:, 128:128 + NE - 2], 0.0, None, ALU.mult), reads=[bkey], writes=["cntf3"])
            op("dve", lambda e: e.reduce_max(rt_s[:, 13:14], bank[:, 128:128 + NE], AX.X), reads=[bkey], writes=["rt13"])
            op("dve", lambda e: e.tensor_scalar(cntf[:, NE:NE + 1], rt_s[:, 13:14], 512.5, None, ALU.is_gt), reads=["rt13"], writes=["cntf2"])
            op("dve", lambda e: e.tensor_scalar(cntf[:, NE + 1:NE + 2], rt_s[:, 13:14], 1024.5, None, ALU.is_gt), reads=["rt13"], writes=["cntf4"])
            op("dve", lambda e: e.tensor_copy(flag_i[:], cntf[:]), reads=["cntf", "cntf2", "cntf3", "cntf4"], writes=["flag"])

            ystore_keys = []

            def prefetch_hs(e_, j0):
                r0 = e_ * S + j0 * 128
                hi = e_ % 2
                dma("sp", "hsl%d" % hi, [(hs_tok[hi], Hs_d[r0:r0 + 256, :].rearrange("(j p) d -> p j d", p=128))],
                    reads=scat_keys, writes=[("hst", hi)])
                return hi

            def expert_tiles(e_, wi, j0, hi=None):
                r0 = e_ * S + j0 * 128
                if hi is None:
                    hi = prefetch_hs(e_, j0)
                he = nxt("hte")
                for j in range(2):
                    def tr(e, j=j, hi=hi):
                        ins = None
                        for c in range(8):
                            ins = e.transpose(pTr[:, c, :], hs_tok[hi][:, j, c * 128:(c + 1) * 128], ident[:])
                        return ins
                    op("pe", tr, reads=[("hst", hi), "ident"], writes=[BK_TR])
                    op("act", lambda e, j=j, he=he: e.copy(hTe[he][:, :, j * 128:(j + 1) * 128], pTr[:]),
                       reads=[BK_TR], writes=[("hte", he, j)])
                hk = [("hte", he, 0), ("hte", he, 1)]
                hi_ = nxt("hid")
                for dc in range(4):
                    gb = nxt("proj")
                    gbank, gkey = B[gb]
                    ui = nxt("st")
                    ubank, ukey = (pY[0][:, ui * 512:ui * 512 + 256], Yk[0][ui])

                    def mmg(e, dc=dc, gbank=gbank):
                        ins = None
                        for kc in range(8):
                            ins = e.matmul(gbank[:, 0:256], Wg[wi][:, kc, dc * 128:(dc + 1) * 128], hTe[he][:, kc, :],
                                           start=(kc == 0), stop=(kc == 7))
                        return ins

                    def mmu(e, dc=dc, ubank=ubank):
                        ins = None
                        for kc in range(8):
                            ins = e.matmul(ubank, Wu[wi][:, kc, dc * 128:(dc + 1) * 128], hTe[he][:, kc, :],
                                           start=(kc == 0), stop=(kc == 7))
                        return ins
                    op("pe", mmg, reads=[("We", wi)] + hk, writes=[gkey])
                    op("pe", mmu, reads=[("We", wi)] + hk, writes=[ukey])
                    si = nxt("sg")
                    op("act", lambda e, si=si, gbank=gbank: e.activation(sg[si][:, 0:256], gbank[:, 0:256], AF.Silu),
                       reads=[gkey], writes=[("sg", si)])
                    op("dve", lambda e, si=si, ubank=ubank, dc=dc: e.tensor_tensor(hid[hi_][:, dc, 0:256], ubank, sg[si][:, 0:256], ALU.mult),
                       reads=[ukey, ("sg", si)], writes=[("hid", hi_, dc)])
                for j in range(2):
                    def mmd(e, j=j):
                        ins = None
                        for half in range(2):
                            for dc in range(4):
                                ins = e.matmul(pY[1][:, half * 512:(half + 1) * 512], hid[hi_][:, dc, j * 128:(j + 1) * 128],
                                               Wd[wi][:, dc, half * 512:(half + 1) * 512], start=(dc == 0), stop=(dc == 3))
                        return ins
                    op("pe", mmd, reads=[("We", wi)] + [("hid", hi_, dc) for dc in range(4)], writes=list(Yk[1]))
                    yo = nxt("yo")
                    if j == 0:
                        op("act", lambda e, yo=yo: e.copy(yout[yo], pY[1][:]), reads=list(Yk[1]), writes=[("yo", yo)])
                    else:
                        op("dve", lambda e, yo=yo: e.tensor_copy(yout[yo], pY[1][:]), reads=list(Yk[1]), writes=[("yo", yo)])
                    rr0 = r0 + j * 128
                    dma("sp", "ys%d" % yo, [(Ys_d[rr0:rr0 + 128, :], yout[yo])], reads=[("yo", yo)], writes=[("Ys", e_, j0 + j)])
                    ystore_keys.append(("Ys", e_, j0 + j))

            hi_next = prefetch_hs(0, 0)
            for e_ in range(n_experts):
                wi = wi_next
                hi_cur = hi_next
                if e_ + 1 < n_experts:
                    wi_next = load_e(e_ + 1)
                    hi_next = prefetch_hs(e_ + 1, 0)
                expert_tiles(e_, wi, 0, hi_cur)
                if guard:
                    sc.begin_guard(flag_i[0:1, e_:e_ + 1], "flag")
                    expert_tiles(e_, wi, 2)
                    sc.end_guard()
            if guard:
                for fcol, jlo, jhi in ((NE, 4, 8), (NE + 1, 8, NT)):
                    sc.begin_guard(flag_i[0:1, fcol:fcol + 1], "flag")
                    wi_next = load_e(0)
                    for e_ in range(n_experts):
                        wi = wi_next
                        if e_ + 1 < n_experts:
                            wi_next = load_e(e_ + 1)
                        for j0 in range(jlo, jhi, 2):
                            expert_tiles(e_, wi, j0)
                    sc.end_guard()
            ykeys = list(dict.fromkeys(ystore_keys))
            for t in range(NT):
                for k in range(2):
                    yg = nxt("yg")

                    def g_fn(e, sem, t=t, k=k, yg=yg):
                        e.indirect_dma_start(out=Yg[yg][:, :], out_offset=None, in_=Ys_d[:, :],
                                             in_offset=bass.IndirectOffsetOnAxis(ap=slot01[:, t, k:k + 1], axis=0)).then_inc(sem, 16)
                    op("pool", g_fn, reads=ykeys + [("slot", t)], writes=[("yg", yg)], dma_key="yg%d" % yg, ndma=1)
                    op("dve", lambda e, t=t, k=k, yg=yg: e.scalar_tensor_tensor(x_sb[:, t, :], Yg[yg], g01[:, t, k:k + 1], x_sb[:, t, :],
                                                                                ALU.mult, ALU.add),
                       reads=[("yg", yg), ("x", t), ("g0", t), ("g1", t)], writes=[("x", t)])

        def barrier():
            evs = []
            for name, E in sc.eng.items():
                if E["count"] > 0:
                    evs.append((name, E["sem"], E["count"], name))
            for k, d in sc.dsem.items():
                evs.append(("d_" + str(k), d[0], d[1], "dma"))
            for name in ("pe", "act", "dve", "pool", "sp"):
                sc.wait_all(name, [ev for ev in evs if ev[0] != name])

        store_events = []
        for s in range(nseq):
            dma("sp", "xin", [(x_sb[:, 4 * g:4 * g + 4, :], x_d[s, 512 * g:512 * g + 512, :].rearrange("(t p) d -> p t d", p=128))
                              for g in range(4)],
                writes=[("x", t) for t in range(NT)])
            for l in range(depth):
                if do_attn:
                    barrier()
                    attn_phase(s, l)
                if do_moe:
                    barrier()
                    if sparse:
                        moe_phase_sparse(s, l)
                    else:
                        moe_phase(s, l)
            ev = dma("sp", "xout", [(y_d[s, 512 * g:512 * g + 512, :].rearrange("(t p) d -> p t d", p=128), x_sb[:, 4 * g:4 * g + 4, :])
                                    for g in range(4)],
                     reads=[("x", t) for t in range(NT)])
            store_events.append(ev)
        sc.wait_all("sp", store_events)
        sc.emit()
    nc._in_names = list(dr)
    return nc


_CACHE = {}


def kernel(**inputs):
    shared = _host_prep(inputs)
    x = np.ascontiguousarray(np.asarray(inputs["x"], dtype=np.float32))
    mem = np.ascontiguousarray(np.asarray(inputs["mem"], dtype=np.float32))
    if "nc" not in _CACHE:
        _CACHE["nc"] = build_nc()
    nc = _CACHE["nc"]
    in_maps = []
    for c in range(N_CORES):
        m = dict(shared)
        m["x"] = x[c * NSEQ:(c + 1) * NSEQ]
        m["mem"] = mem[c * NSEQ:(c + 1) * NSEQ]
        in_maps.append(m)
    res = run_bass_kernel_spmd(nc, in_maps, core_ids=list(range(N_CORES)))
    out = np.concatenate([np.asarray(r["y"]) for r in res.results], axis=0)
    return out.astype(np.float32)
```
